# Optimizing a Trainium2 kernel written in Bass

```python
import math
import jax, jax.numpy as jnp
from jax import lax
import numpy as np

D_MODEL = 1024
BATCH = 4
SEQ = 8192
DEPTH = 2

CTX_LEN = 256
GRID_W = 64
SGU_CHUNK = 128
SGU_ROWS_PER_CHUNK = SGU_CHUNK // GRID_W
SGU_WIDTH = D_MODEL // 2
SGU_GROUPS = 4
SGU_GROUP_DIM = SGU_WIDTH // SGU_GROUPS
GDN_HEADS = 4
GDN_HEAD_DIM = 128
GDN_WIDTH = GDN_HEADS * GDN_HEAD_DIM
GDN_CHUNK = 64
CONV_K = 5
D_FF = 3584
N_EXPERTS = 8
TOP_K = 2
EXPERT_FF = 3584
N_DENSE = (DEPTH + 1) // 2
N_MOE = DEPTH // 2
ALPHA = (2.0 * DEPTH) ** 0.25
BETA = (8.0 * DEPTH) ** -0.25
LN_EPS = 1e-6
IN_SIZES = (SGU_WIDTH, SGU_WIDTH, 3 * GDN_WIDTH, GDN_WIDTH, GDN_HEADS, GDN_HEADS, GDN_HEADS, GDN_HEADS, D_MODEL, D_MODEL)
N_IN = 2 * SGU_WIDTH + 4 * GDN_WIDTH + 4 * GDN_HEADS + 2 * D_MODEL

kernel_name = 'hybrid_sgu_gdn_moe_prefix_block'


def layer_norm(x, g=None, b=None):
    xf = x.astype(jnp.float32)
    mu = jnp.mean(xf, axis=-1, keepdims=True)
    var = jnp.mean(jnp.square(xf - mu), axis=-1, keepdims=True)
    y = (xf - mu) * lax.rsqrt(var + LN_EPS)
    if g is not None:
        y = y * g.astype(jnp.float32) + b.astype(jnp.float32)
    return y.astype(x.dtype)


def modulate(x, shift, scale):
    return layer_norm(x) * (1.0 + scale) + shift


def split_in(p):
    idx = [int(i) for i in np.cumsum(IN_SIZES)[:-1]]
    return jnp.split(p, idx, axis=-1)


def l2norm(x):
    xf = x.astype(jnp.float32)
    return xf * lax.rsqrt(jnp.sum(xf * xf, axis=-1, keepdims=True) + LN_EPS)


def short_conv(x, w):
    pad = w.shape[0] // 2
    return lax.conv_general_dilated(x, w[:, None, :].astype(x.dtype), window_strides=(1,),
                                    padding=[(pad, pad)], dimension_numbers=('NWC', 'WIO', 'NWC'),
                                    feature_group_count=x.shape[-1])


def sgu(u, v, ln_g, ln_b, w_s, b_s, n_chunks):
    bsz = u.shape[0]
    vn = layer_norm(v, ln_g, ln_b).reshape(bsz, n_chunks, SGU_CHUNK, SGU_GROUPS, SGU_GROUP_DIM)
    mixed = jnp.einsum('gpq,bnqgc->bnpgc', w_s, vn) + b_s.T[:, :, None]
    return u * mixed.reshape(u.shape)


def gated_delta_chunked(q, k, v, log_g, beta, s0):
    bsz, t, h, dk = q.shape
    dv = v.shape[-1]
    n = t // GDN_CHUNK
    f32 = jnp.float32

    def blocks(a):
        a = a.astype(f32).reshape((bsz, n, GDN_CHUNK, h) + a.shape[3:])
        return jnp.moveaxis(a, 3, 1)

    qb, kb, vb = blocks(q), blocks(k), blocks(v)
    gb, bb = blocks(log_g), blocks(beta)
    gam = jnp.cumsum(gb, axis=-1)
    pos = jnp.arange(GDN_CHUNK)
    incl = pos[:, None] >= pos[None, :]
    strict = pos[:, None] > pos[None, :]
    decay = jnp.exp(jnp.where(incl, gam[..., :, None] - gam[..., None, :], -jnp.inf))
    kk = jnp.einsum('bhncd,bhnsd->bhncs', kb, kb)
    lower = jnp.where(strict, bb[..., :, None] * kk * decay, 0.0) + jnp.eye(GDN_CHUNK, dtype=f32)
    rhs = jnp.concatenate([bb[..., None] * vb, (bb * jnp.exp(gam))[..., None] * kb], axis=-1)
    sol = lax.linalg.triangular_solve(lower, rhs, left_side=True, lower=True, unit_diagonal=True)
    u, wk = sol[..., :dv], sol[..., dv:]
    qk = jnp.einsum('bhncd,bhnsd->bhncs', qb, kb) * decay
    qg = qb * jnp.exp(gam)[..., None]
    kd = kb * jnp.exp(gam[..., -1:] - gam)[..., None]
    gc = jnp.exp(gam[..., -1])
    xs = tuple(jnp.moveaxis(a, 2, 0) for a in (u, wk, qk, qg, kd, gc))

    def step(s, inp):
        u_n, wk_n, qk_n, qg_n, kd_n, gc_n = inp
        w = u_n - jnp.einsum('bhcd,bhdv->bhcv', wk_n, s)
        o = jnp.einsum('bhcd,bhdv->bhcv', qg_n, s) + jnp.einsum('bhcs,bhsv->bhcv', qk_n, w)
        s = gc_n[..., None, None] * s + jnp.einsum('bhcd,bhcv->bhdv', kd_n, w)
        return s, o

    s_fin, o = lax.scan(step, s0.astype(f32), xs)
    o = jnp.transpose(o, (1, 0, 3, 2, 4)).reshape(bsz, t, h, dv)
    return o.astype(v.dtype), s_fin


def token_mixer(h_lat, h_ctx, n_chunks_lat, n_chunks_ctx, w_in, sgu_ln_g, sgu_ln_b, sgu_w, sgu_b,
                conv_w, a_log, dt_bias, norm_w, w_pa, w_pb, w_o, need_ctx):
    f32 = jnp.float32
    u_l, v_l, qkv_l, z_l, af_l, bf_l, ab_l, bb_l, ga_l, gb_l = split_in(h_lat @ w_in)
    u_c, v_c, qkv_c, z_c, af_c, bf_c, ab_c, bb_c, ga_c, gb_c = split_in(h_ctx @ w_in)

    def qkv_heads(qkv):
        qkv = jax.nn.silu(short_conv(qkv, conv_w))
        q, k, v = jnp.split(qkv, 3, axis=-1)
        shape = qkv.shape[:2] + (GDN_HEADS, GDN_HEAD_DIM)
        q = l2norm(q.reshape(shape)) * (GDN_HEAD_DIM ** -0.5)
        k = l2norm(k.reshape(shape))
        return q, k, v.reshape(shape)

    def gates(a, b, d):
        lg = -jnp.exp(a_log[d].astype(f32)) * jax.nn.softplus(a.astype(f32) + dt_bias[d].astype(f32))
        return lg, jax.nn.sigmoid(b.astype(f32))

    def flip(t):
        return jnp.flip(t, axis=1)

    def gated_out(o, z):
        of = o.astype(f32)
        of = of * lax.rsqrt(jnp.mean(of * of, axis=-1, keepdims=True) + LN_EPS) * norm_w.astype(f32)
        return (of.reshape(z.shape) * jax.nn.silu(z.astype(f32))).astype(z.dtype)

    def merge(o_a, o_b, g_a, g_b):
        y = jax.nn.sigmoid(g_a) * (o_a @ w_pa) + jax.nn.sigmoid(g_b) * (o_b @ w_pb)
        return y @ w_o

    qc, kc, vc = qkv_heads(qkv_c)
    ql, kl, vl = qkv_heads(qkv_l)
    gfc, bfc = gates(af_c, bf_c, 0)
    gbc, bbc = gates(ab_c, bb_c, 1)
    gfl, bfl = gates(af_l, bf_l, 0)
    gbl, bbl = gates(ab_l, bb_l, 1)
    s_zero = jnp.zeros((h_ctx.shape[0], GDN_HEADS, GDN_HEAD_DIM, GDN_HEAD_DIM), f32)
    o_cf, s_cf = gated_delta_chunked(qc, kc, vc, gfc, bfc, s_zero)
    o_cb, s_cb = gated_delta_chunked(flip(qc), flip(kc), flip(vc), flip(gbc), flip(bbc), s_zero)
    o_lf, _ = gated_delta_chunked(ql, kl, vl, gfl, bfl, s_cf)
    o_lb, _ = gated_delta_chunked(flip(ql), flip(kl), flip(vl), flip(gbl), flip(bbl), s_cb)

    oa_l = sgu(jax.nn.gelu(u_l, approximate=False), jax.nn.gelu(v_l, approximate=False),
               sgu_ln_g, sgu_ln_b, sgu_w, sgu_b, n_chunks_lat)
    y_lat = merge(oa_l, gated_out(o_lf + flip(o_lb), z_l), ga_l, gb_l)
    y_ctx = None
    if need_ctx:
        oa_c = sgu(jax.nn.gelu(u_c, approximate=False), jax.nn.gelu(v_c, approximate=False),
                   sgu_ln_g, sgu_ln_b, sgu_w, sgu_b, n_chunks_ctx)
        y_ctx = merge(oa_c, gated_out(o_cf + flip(o_cb), z_c), ga_c, gb_c)
    return y_lat, y_ctx


def swiglu(h, w1, w3, w2):
    return (jax.nn.silu(h @ w1) * (h @ w3)) @ w2


def moe_ffn(h, router, w1, w3, w2):
    logits = jnp.einsum('btd,de->bte', h, router).astype(jnp.float32)
    top_val, top_idx = lax.top_k(logits, TOP_K)
    top_w = jax.nn.softmax(top_val, axis=-1)
    gate = jnp.sum(jax.nn.one_hot(top_idx, N_EXPERTS, dtype=jnp.float32) * top_w[..., None], axis=-2)
    gate = gate.astype(h.dtype)
    y = jnp.zeros_like(h)
    for e in range(N_EXPERTS):
        y = y + gate[..., e:e + 1] * swiglu(h, w1[e], w3[e], w2[e])
    return y


def setup_inputs(seed: int = 0) -> dict:
    key = jax.random.key(seed)
    ks = iter(jax.random.split(key, 40))
    nrm = lambda shape: jax.random.normal(next(ks), shape, jnp.float32)
    dt = jnp.exp(jax.random.uniform(next(ks), (DEPTH, 2, GDN_HEADS), jnp.float32,
                                    math.log(1e-3), math.log(1e-1)))
    return {
        'x': nrm((BATCH, SEQ, D_MODEL)),
        'c': nrm((BATCH, D_MODEL)),
        'ctx': nrm((BATCH, CTX_LEN, D_MODEL)),
        'c_ctx': nrm((D_MODEL,)),
        'w_ada': nrm((DEPTH, D_MODEL, 6 * D_MODEL)) * D_MODEL ** -0.5,
        'b_ada': 0.02 * nrm((DEPTH, 6 * D_MODEL)),
        'w_in': nrm((DEPTH, D_MODEL, N_IN)) * D_MODEL ** -0.5,
        'sgu_ln_g': 1.0 + 0.02 * nrm((DEPTH, SGU_WIDTH)),
        'sgu_ln_b': 0.02 * nrm((DEPTH, SGU_WIDTH)),
        'sgu_w': nrm((DEPTH, SGU_GROUPS, SGU_CHUNK, SGU_CHUNK)) * SGU_CHUNK ** -0.5,
        'sgu_b': 1.0 + 0.02 * nrm((DEPTH, SGU_GROUPS, SGU_CHUNK)),
        'conv_w': nrm((DEPTH, CONV_K, 3 * GDN_WIDTH)) * CONV_K ** -0.5,
        'a_log': jnp.log(jax.random.uniform(next(ks), (DEPTH, 2, GDN_HEADS), jnp.float32, 1.0, 16.0)),
        'dt_bias': dt + jnp.log(-jnp.expm1(-dt)),
        'gdn_norm_w': 1.0 + 0.02 * nrm((DEPTH, GDN_HEAD_DIM)),
        'w_pa': nrm((DEPTH, SGU_WIDTH, D_MODEL)) * (SGU_WIDTH ** -0.5 * BETA),
        'w_pb': nrm((DEPTH, GDN_WIDTH, D_MODEL)) * (GDN_WIDTH ** -0.5 * BETA),
        'w_o': nrm((DEPTH, D_MODEL, D_MODEL)) * (D_MODEL ** -0.5 * BETA),
        'ln1_g': 1.0 + 0.02 * nrm((DEPTH, D_MODEL)),
        'ln1_b': 0.02 * nrm((DEPTH, D_MODEL)),
        'ln2_g': 1.0 + 0.02 * nrm((DEPTH, D_MODEL)),
        'ln2_b': 0.02 * nrm((DEPTH, D_MODEL)),
        'ffn_w1': nrm((N_DENSE, D_MODEL, D_FF)) * D_MODEL ** -0.5,
        'ffn_w3': nrm((N_DENSE, D_MODEL, D_FF)) * D_MODEL ** -0.5,
        'ffn_w2': nrm((N_DENSE, D_FF, D_MODEL)) * (D_FF ** -0.5 * BETA),
        'moe_router': nrm((N_MOE, D_MODEL, N_EXPERTS)) * D_MODEL ** -0.5,
        'moe_w1': nrm((N_MOE, N_EXPERTS, D_MODEL, EXPERT_FF)) * D_MODEL ** -0.5,
        'moe_w3': nrm((N_MOE, N_EXPERTS, D_MODEL, EXPERT_FF)) * D_MODEL ** -0.5,
        'moe_w2': nrm((N_MOE, N_EXPERTS, EXPERT_FF, D_MODEL)) * (EXPERT_FF ** -0.5 * BETA),
    }


def reference(x, c, ctx, c_ctx, w_ada, b_ada, w_in, sgu_ln_g, sgu_ln_b, sgu_w, sgu_b, conv_w, a_log,
              dt_bias, gdn_norm_w, w_pa, w_pb, w_o, ln1_g, ln1_b, ln2_g, ln2_b, ffn_w1, ffn_w3, ffn_w2,
              moe_router, moe_w1, moe_w3, moe_w2):
    rows = x.shape[1] // GRID_W
    n_chunks_lat = rows // SGU_ROWS_PER_CHUNK
    n_chunks_ctx = ctx.shape[1] // SGU_CHUNK
    silu_c = jax.nn.silu(c)
    silu_cc = jax.nn.silu(c_ctx)
    x_lat, x_ctx = x, ctx
    for l in range(DEPTH):
        need_ctx = l < DEPTH - 1
        m_lat = jnp.split((silu_c @ w_ada[l] + b_ada[l])[:, None, :], 6, axis=-1)
        m_ctx = jnp.split(silu_cc @ w_ada[l] + b_ada[l], 6, axis=-1)
        h_lat = modulate(x_lat, m_lat[0], m_lat[1])
        h_ctx = modulate(x_ctx, m_ctx[0], m_ctx[1])
        y_lat, y_ctx = token_mixer(h_lat, h_ctx, n_chunks_lat, n_chunks_ctx, w_in[l], sgu_ln_g[l],
                                   sgu_ln_b[l], sgu_w[l], sgu_b[l], conv_w[l], a_log[l], dt_bias[l],
                                   gdn_norm_w[l], w_pa[l], w_pb[l], w_o[l], need_ctx)
        x_lat = layer_norm(ALPHA * x_lat + m_lat[2] * y_lat, ln1_g[l], ln1_b[l])
        if need_ctx:
            x_ctx = layer_norm(ALPHA * x_ctx + m_ctx[2] * y_ctx, ln1_g[l], ln1_b[l])
        h_lat = modulate(x_lat, m_lat[3], m_lat[4])
        if l % 2 == 0:
            f_lat = swiglu(h_lat, ffn_w1[l // 2], ffn_w3[l // 2], ffn_w2[l // 2])
        else:
            f_lat = moe_ffn(h_lat, moe_router[l // 2], moe_w1[l // 2], moe_w3[l // 2], moe_w2[l // 2])
        if need_ctx:
            h_ctx = modulate(x_ctx, m_ctx[3], m_ctx[4])
            if l % 2 == 0:
                f_ctx = swiglu(h_ctx, ffn_w1[l // 2], ffn_w3[l // 2], ffn_w2[l // 2])
            else:
                f_ctx = moe_ffn(h_ctx, moe_router[l // 2], moe_w1[l // 2], moe_w3[l // 2], moe_w2[l // 2])
            x_ctx = layer_norm(ALPHA * x_ctx + m_ctx[5] * f_ctx, ln2_g[l], ln2_b[l])
        x_lat = layer_norm(ALPHA * x_lat + m_lat[5] * f_lat, ln2_g[l], ln2_b[l])
    return x_lat
```

```python
import numpy as np
import concourse.bass as bass
import concourse.mybir as mybir
from concourse.bass_utils import run_bass_kernel_spmd
from contextlib import ExitStack

F32 = mybir.dt.float32
BF16 = mybir.dt.bfloat16
ALU = mybir.AluOpType
AF = mybir.ActivationFunctionType
AX = mybir.AxisListType

D = 1024
TC = 256
NIN = 5136
DFF = 3584
NE = 8
LN_EPS = 1e-6
DEPTH = 2
ALPHA = (2.0 * DEPTH) ** 0.25


class Buf:
    __slots__ = ("name", "w", "r")

    def __init__(self, name):
        self.name = name
        self.w = None
        self.r = {}


class Sch:
    ENG = ("pe", "dve", "act", "pool", "sp")
    NDMA = 40
    NSW = 8

    def __init__(self, nc, stack, needed=None):
        self.nc = nc
        self.e = {"pe": nc.tensor, "dve": nc.vector, "act": nc.scalar, "pool": nc.gpsimd, "sp": nc.sync}
        self.sem = {}
        self.cnt = {}
        for k in self.ENG:
            self.sem[k] = stack.enter_context(nc.semaphore("sem_" + k))
            self.cnt[k] = 0
        for j in range(self.NDMA):
            k = "d%d" % j
            self.sem[k] = stack.enter_context(nc.semaphore("sem_" + k))
            self.cnt[k] = 0
        self.rr = 0
        self.rr_sw = 0
        self.waited = {k: {} for k in self.ENG}
        self.nops = 0
        self.nwaits = 0
        self.ninst = {k: 0 for k in self.ENG}
        self.record = needed is None
        self.needed = {k: set() for k in self.ENG} if needed is None else needed
        if not self.record:
            self.rank = {}
            for k in self.ENG:
                srt = sorted(self.needed[k])
                self.rank[k] = {v: i + 1 for i, v in enumerate(srt)}

    def _wait(self, eng, key, val):
        if val <= 0:
            return
        if self.waited[eng].get(key, 0) >= val:
            return
        self.waited[eng][key] = val
        self.nwaits += 1
        self.ninst[eng] += 1
        if key in self.needed:
            if self.record:
                self.needed[key].add(val)
                return
            sv = self.rank[key][val]
        else:
            sv = val
        self.e[eng].wait_ge(self.sem[key], sv)

    def op(self, eng, fn, reads=(), writes=(), dma=False):
        deps = {}
        for b in reads:
            if b.w is not None:
                k, v = b.w
                if deps.get(k, 0) < v:
                    deps[k] = v
        for b in writes:
            if b.w is not None:
                k, v = b.w
                if deps.get(k, 0) < v:
                    deps[k] = v
            for k, v in b.r.items():
                if deps.get(k, 0) < v:
                    deps[k] = v
        for k, v in deps.items():
            if eng == "pe" and k == "pe" and not dma:
                continue
            self._wait(eng, k, v)
        if dma:
            if eng == "pool":
                j = self.NDMA - self.NSW + self.rr_sw
                self.rr_sw = (self.rr_sw + 1) % self.NSW
            else:
                j = self.rr
                self.rr = (self.rr + 1) % (self.NDMA - self.NSW)
            key = "d%d" % j
            self._wait(eng, key, self.cnt[key])
            inst = fn()
            inst.then_inc(self.sem[key], 16)
            self.cnt[key] += 16
            tok = (key, self.cnt[key])
        else:
            inst = fn()
            self.cnt[eng] += 1
            if (not self.record) and (self.cnt[eng] in self.rank[eng]):
                inst.then_inc(self.sem[eng], 1)
            tok = (eng, self.cnt[eng])
        for b in writes:
            b.w = tok
            b.r = {}
        for b in reads:
            k, v = tok
            if b.r.get(k, 0) < v:
                b.r[k] = v
        self.nops += 1
        self.ninst[eng] += 1
        return inst

    def barrier(self):
        for eng in self.ENG:
            for k, v in self.cnt.items():
                if k == eng:
                    continue
                self._wait(eng, k, v)

    def final_wait(self, eng="sp"):
        for k, v in self.cnt.items():
            if k != eng:
                self._wait(eng, k, v)


class Ctx:
    def __getattr__(self, name):
        if name.startswith("b_"):
            b = Buf(name)
            object.__setattr__(self, name, b)
            return b
        raise AttributeError(name)


def bc(ap, shape):
    return ap.broadcast_to(list(shape))


def rstd_op(C, out, in_, buf, eps=LN_EPS, extra_reads=()):
    nc, S = C.nc, C.S
    S.op("act", lambda: nc.scalar.activation(out=out, in_=in_, func=AF.Sqrt, bias=C.epsc[:in_.shape[0], 0:1], scale=1.0),
         reads=[buf, C.cstb] + list(extra_reads), writes=[buf])
    S.op("dve", lambda: nc.vector.reciprocal(out=out, in_=out), reads=[buf], writes=[buf])


ALL_STEPS = [(p, l) for l in range(DEPTH) for p in ("mod", "a", "b", "cd", "e")]
PLANS = [
    dict(steps=[("mod", 0), ("a", 0), ("b", 0), ("cd", 0)], ext_in=[], ext_out=["oaT0", "sgT0", "zs0", "of0", "ob0"], wconv=[]),
    dict(steps=[("mod", 0), ("e", 0), ("mod", 1), ("a", 1), ("b", 1), ("cd", 1)],
         ext_in=["oaT0", "sgT0", "zs0", "of0", "ob0"], ext_out=["x1", "oaT1", "sgT1", "zs1", "of1", "ob1"], wconv=[0]),
    dict(steps=[("mod", 1), ("e", 1)], ext_in=["x1", "oaT1", "sgT1", "zs1", "of1", "ob1"], ext_out=[],
         wconv=list(range(1, NE + 1))),
]


def build(TL, debug=False, stop_after=None, ne=NE, plan=None):
    if plan is None:
        steps = []
        for st in ALL_STEPS:
            steps.append(st)
            if stop_after is not None and st == stop_after:
                break
        plan = dict(steps=steps, ext_in=[], ext_out=[], wconv=list(range(ne + 1)))
    _, C1 = build1(TL, debug, ne, plan, None)
    return build1(TL, debug, ne, plan, C1.S.needed)


class Lazy:
    def __init__(self, make):
        object.__setattr__(self, "_make", make)

    def __getattr__(self, name):
        v = self._make(name)
        object.__setattr__(self, name, v)
        return v


def build1(TL, debug, ne, plan, needed):
    T = TC + TL
    OWN = TL // 2
    NCH = T // 64
    nc = bass.Bass("TRN2", target_bir_lowering=False)
    C = Ctx()
    C.nc = nc
    C.TL, C.T, C.OWN, C.NCH = TL, T, OWN, NCH
    C.debug = debug
    C.ne = ne
    C.used_in = []
    C.ext_in = list(plan["ext_in"])
    C.ext_out = list(plan["ext_out"])
    in_specs = {
        "x": ([T, D], F32), "sc": ([128, 8, 2], F32), "w_ada": ([2, D, 6 * D], F32), "b_ada_fm": ([2, 128, 48], F32),
        "b_ada_bc": ([2, 128, 6 * D], F32), "w_in": ([2, D, NIN], F32), "sgu_ln": ([2, 128, 2, 512], F32),
        "sgu_wT": ([2, 128, 4, 128], F32), "sgu_b": ([2, 128, 4, 128], F32), "conv_w": ([2, 128, 12, 5], F32),
        "gate_par": ([2, 128, 2, 8], F32), "gdn_nw": ([2, 128, 512], F32), "w_pa": ([2, 512, D], F32),
        "w_pb": ([2, 512, D], F32), "w_o": ([2, D, D], F32), "lnp": ([2, 128, 4, D], F32),
        "ffn_w1": ([D, DFF], F32), "ffn_w3": ([D, DFF], F32), "ffn_w2": ([DFF, D], F32), "router": ([128, 8, NE], F32),
        "moe_w1": ([ne, D, DFF], F32), "moe_w3": ([ne, D, DFF], F32), "moe_w2": ([ne, DFF, D], F32),
        "consts": ([128, 8, 128], F32),
    }

    def make_in(name):
        shape, dt = in_specs[name]
        C.used_in.append(name)
        return nc.dram_tensor(name, list(shape), dt, kind="ExternalInput").ap()

    C.I = Lazy(make_in)
    sc_specs = {
        "x1": ([T, D], F32), "qkvT": ([12, 128, T], F32), "qkn": ([8, 128, T], F32), "ktok": ([T, 512], F32),
        "vtok": ([T, 512], F32), "glb": ([T, 16], F32), "oaT": ([4, 128, T], BF16), "sgT": ([16, 128, T], BF16),
        "zs": ([T, 512], F32), "of": ([T, 512], F32), "ob": ([T, 512], F32),
        "wb1": ([ne + 1, D, DFF], BF16), "wb3": ([ne + 1, D, DFF], BF16), "wb2": ([ne + 1, DFF, D], BF16),
    }
    shared = {}
    C.ext_names = {}

    def make_sc(l):
        def mk(name):
            key = name if name in ("x1", "wb1", "wb3", "wb2") else name + str(l)
            if key in shared:
                return shared[key]
            shape, dt = sc_specs[name]
            if key in C.ext_in:
                kind = "ExternalInput"
            elif key in C.ext_out or (debug and not name.startswith("wb")):
                kind = "ExternalOutput"
            else:
                kind = "Internal"
            t = nc.dram_tensor("s_" + key, list(shape), dt, kind=kind).ap()
            shared[key] = t
            C.ext_names[key] = "s_" + key
            return t
        return mk

    C.SxL = [Lazy(make_sc(l)) for l in range(DEPTH)]
    C.Sx = C.SxL[0]
    steps = plan["steps"]
    if ("e", DEPTH - 1) in steps:
        C.y_out = nc.dram_tensor("y", [OWN, D], F32, kind="ExternalOutput").ap()

    with ExitStack() as stack:
        S = Sch(nc, stack, needed)
        C.S = S
        C.stack = stack
        C.ps = [stack.enter_context(nc.psum_tensor("ps%d" % i, [128, 512], F32)) for i in range(8)]
        C.psb = [Buf("ps%d" % i) for i in range(8)]
        C.cst = stack.enter_context(nc.sbuf_tensor("cst", [128, 8, 128], F32))
        C.cstb = Buf("cst")
        S.op("sp", lambda: nc.sync.dma_start(out=C.cst[:], in_=C.I.consts[:, :, :]), writes=[C.cstb], dma=True)
        C.epsc = stack.enter_context(nc.sbuf_tensor("epsc", [128, 1], F32))
        S.op("dve", lambda: nc.vector.memset(C.epsc[:], LN_EPS), writes=[C.cstb])

        C.wb = [Buf("wb%d" % e) for e in range(ne + 1)]
        weight_convert(C, [e for e in plan["wconv"] if e <= ne])
        fns = {"mod": modulation, "a": pass_a, "b": pass_b, "cd": pass_cd, "e": pass_e}
        for (p, l) in steps:
            C.Sx = C.SxL[l]
            with ExitStack() as ls:
                C.ls = ls
                fns[p](C, l)
                S.barrier()
        import os
        for _i in range(int(os.environ.get("EXTRA_ACT", "0"))):
            S.op("act", lambda: nc.scalar.copy(out=C.epsc[:, 0:1], in_=C.epsc[:, 0:1]), reads=[], writes=[])
        for _i in range(int(os.environ.get("EXTRA_DVE", "0"))):
            S.op("dve", lambda: nc.vector.tensor_copy(C.epsc[:, 0:1], C.epsc[:, 0:1]), reads=[], writes=[])
        S.final_wait("sp")
        S.final_wait("act")
    C.nc = nc
    return nc, C


def weight_convert(C, which):
    nc, S, I, Sx = C.nc, C.S, C.I, C.Sx
    for e in which:
        if e == 0:
            a, b, c = I.ffn_w1, I.ffn_w3, I.ffn_w2
        else:
            a, b, c = I.moe_w1[e - 1], I.moe_w3[e - 1], I.moe_w2[e - 1]
        for (dst, src, rows) in ((Sx.wb1[e], a, D), (Sx.wb3[e], b, D), (Sx.wb2[e], c, DFF)):
            step = 512
            for r0 in range(0, rows, step):
                S.op("pool", lambda dst=dst, src=src, r0=r0, step=step: nc.gpsimd.dma_start(
                    out=dst[r0:r0 + step, :], in_=src[r0:r0 + step, :]), writes=[C.wb[e]], dma=True)


def modulation(C, l):
    nc, S, I = C.nc, C.S, C.I
    ls = C.ls
    if not hasattr(C, "mod_fm"):
        st = C.stack
        C.mod_fm = st.enter_context(nc.sbuf_tensor("mod_fm", [128, 48, 2], F32))
        C.mod_fmb = Buf("mod_fm")
        C.mod_bc = st.enter_context(nc.sbuf_tensor("mod_bc", [128, 2, 2, D], F32))
        C.mod_bcb = Buf("mod_bc")
    C.sil = ls.enter_context(nc.sbuf_tensor("sil%d" % l, [128, 8, 2], F32))
    C.silb = Buf("sil")
    C.silB = ls.enter_context(nc.sbuf_tensor("silB%d" % l, [128, 8, 2, 128], F32))
    C.silBb = Buf("silB")
    C.bfm = ls.enter_context(nc.sbuf_tensor("bfm%d" % l, [128, 48], F32))
    C.bfmb = Buf("bfm")
    S.op("sp", lambda: nc.sync.dma_start(out=C.sil[:], in_=I.sc[:, :, :]), writes=[C.silb], dma=True)
    S.op("act", lambda: nc.scalar.activation(out=C.sil[:], in_=C.sil[:], func=AF.Silu), reads=[C.silb], writes=[C.silb])
    for w in range(2):
        S.op("dve", lambda w=w: nc.vector.tensor_copy(
            C.silB[:, :, w, :], bc(C.sil[:, :, w:w + 1], [128, 8, 128])), reads=[C.silb], writes=[C.silBb])
    C.wada = [ls.enter_context(nc.sbuf_tensor("wada%d_%d" % (i, l), [128, 8, 512], F32)) for i in range(2)]
    C.wadab = [Buf("wada%d" % i) for i in range(2)]
    S.op("sp", lambda: nc.sync.dma_start(out=C.bfm[:], in_=I.b_ada_fm[l]), writes=[C.bfmb], dma=True)
    for w, which in enumerate((2, 5)):
        S.op("sp", lambda w=w, which=which: nc.sync.dma_start(
            out=C.mod_bc[:, w, 0, :], in_=I.b_ada_bc[l][:, which * D:(which + 1) * D]), writes=[C.mod_bcb], dma=True)
        S.op("sp", lambda w=w, which=which: nc.sync.dma_start(
            out=C.mod_bc[:, w, 1, :], in_=I.b_ada_bc[l][:, which * D:(which + 1) * D]), writes=[C.mod_bcb], dma=True)
    pfm, pbc = C.ps[0], C.ps[1]
    for blk in range(12):
        wt, wtb = C.wada[blk % 2], C.wadab[blk % 2]
        S.op("sp", lambda wt=wt, blk=blk: nc.sync.dma_start(
            out=wt[:], in_=I.w_ada[l][:, blk * 512:(blk + 1) * 512].rearrange("(k p) c -> p k c", p=128)),
            writes=[wtb], dma=True)
        which = blk // 2
        if which in (2, 5):
            w = 0 if which == 2 else 1
            for lc in range(2):
                for k in range(8):
                    S.op("pe", lambda k=k, lc=lc, wt=wt: nc.tensor.matmul(
                        pbc[:, :], C.silB[:, k, lc, :], wt[:, k, :], start=(k == 0), stop=(k == 7)),
                        reads=[C.silBb, wtb], writes=[C.psb[1]])
                S.op("dve", lambda w=w, lc=lc, blk=blk: nc.vector.tensor_tensor(
                    out=C.mod_bc[:, w, lc, (blk % 2) * 512:(blk % 2 + 1) * 512],
                    in0=C.mod_bc[:, w, lc, (blk % 2) * 512:(blk % 2 + 1) * 512], in1=pbc[:, :], op=ALU.add),
                    reads=[C.psb[1], C.mod_bcb], writes=[C.mod_bcb])
        else:
            for cc in range(4):
                j = blk * 4 + cc
                for k in range(8):
                    S.op("pe", lambda k=k, cc=cc, wt=wt: nc.tensor.matmul(
                        pfm[:, cc * 2:cc * 2 + 2], wt[:, k, cc * 128:(cc + 1) * 128], C.sil[:, k, :],
                        start=(k == 0), stop=(k == 7)), reads=[C.silb, wtb], writes=[C.psb[0]])
            for cc in range(4):
                j = blk * 4 + cc
                S.op("dve", lambda j=j, cc=cc: nc.vector.tensor_tensor(
                    out=C.mod_fm[:, j, :], in0=pfm[:, cc * 2:cc * 2 + 2], in1=bc(C.bfm[:, j:j + 1], [128, 2]),
                    op=ALU.add), reads=[C.psb[0], C.bfmb], writes=[C.mod_fmb])
    for j0 in (8, 32):
        S.op("dve", lambda j0=j0: nc.vector.tensor_scalar_add(
            out=C.mod_fm[:, j0:j0 + 8, :], in0=C.mod_fm[:, j0:j0 + 8, :], scalar1=1.0),
            reads=[C.mod_fmb], writes=[C.mod_fmb])


def groups_of(C, gsz):
    g = [(0, 2, 1)]
    r = TC
    while r < C.T:
        n = min(gsz, (C.T - r) // 128)
        g.append((r, n, 0))
        r += n * 128
    return g


def ln_modT(C, l, xs, xsb, nt, which, isctx, hT, hTb, tagsfx, xn, xnb, st6, stb, hT32=None, use_act=True):
    nc, S = C.nc, C.S
    jsh, jsc = which * 8, which * 8 + 8
    for t in range(nt):
        for hh in range(2):
            S.op("dve", lambda t=t, hh=hh: nc.vector.bn_stats(out=st6[:, hh, :], in_=xs[:, t, hh * 512:(hh + 1) * 512]),
                 reads=[xsb], writes=[stb])
        S.op("dve", lambda: nc.vector.bn_aggr(out=st6[:, 2, 0:2], in_=st6[:, 0:2, :]), reads=[stb], writes=[stb])
        rstd_op(C, st6[:, 2, 2:3], st6[:, 2, 1:2], stb)
        S.op("dve", lambda t=t: nc.vector.tensor_scalar(out=xn[:], in0=xs[:, t, :], scalar1=st6[:, 2, 0:1],
                                                        scalar2=st6[:, 2, 2:3], op0=ALU.subtract, op1=ALU.mult),
             reads=[xsb, stb], writes=[xnb])
        for half in range(2):
            pb = 6 + half
            for kk in range(4):
                k = half * 4 + kk
                S.op("pe", lambda k=k, kk=kk, pb=pb: nc.tensor.transpose(
                    C.ps[pb][:, kk * 128:(kk + 1) * 128], xn[:, k * 128:(k + 1) * 128], C.cst[:, 0, :]),
                    reads=[xnb, C.cstb], writes=[C.psb[pb]])
            for kk in range(4):
                k = half * 4 + kk
                eng = "dve" if (kk % 2 == 0 or not use_act) else "act"
                if eng == "dve":
                    S.op("dve", lambda k=k, kk=kk, pb=pb, t=t: nc.vector.tensor_scalar(
                        out=hT[:, k, t * 128:(t + 1) * 128], in0=C.ps[pb][:, kk * 128:(kk + 1) * 128],
                        scalar1=C.mod_fm[:, jsc + k, isctx:isctx + 1], scalar2=C.mod_fm[:, jsh + k, isctx:isctx + 1],
                        op0=ALU.mult, op1=ALU.add), reads=[C.psb[pb], C.mod_fmb], writes=[hTb])
                else:
                    S.op("act", lambda k=k, kk=kk, pb=pb, t=t: nc.scalar.activation(
                        out=hT[:, k, t * 128:(t + 1) * 128], in_=C.ps[pb][:, kk * 128:(kk + 1) * 128],
                        func=AF.Identity, scale=C.mod_fm[:, jsc + k, isctx:isctx + 1],
                        bias=C.mod_fm[:, jsh + k, isctx:isctx + 1]), reads=[C.psb[pb], C.mod_fmb], writes=[hTb])
                if hT32 is not None:
                    S.op("dve", lambda k=k, kk=kk, pb=pb, t=t: nc.vector.tensor_scalar(
                        out=hT32[0][:, k, t * 128:(t + 1) * 128], in0=C.ps[pb][:, kk * 128:(kk + 1) * 128],
                        scalar1=C.mod_fm[:, jsc + k, isctx:isctx + 1], scalar2=C.mod_fm[:, jsh + k, isctx:isctx + 1],
                        op0=ALU.mult, op1=ALU.add), reads=[C.psb[pb], C.mod_fmb], writes=[hT32[1]])


def pass_a(C, l):
    nc, S, I, Sx, ls = C.nc, C.S, C.I, C.Sx, C.ls
    T = C.T
    xsrc = I.x if l == 0 else Sx.x1
    sb = lambda name, shape, dt=F32: ls.enter_context(nc.sbuf_tensor(name + "_a%d" % l, list(shape), dt))
    win = sb("win", [128, 8, NIN], BF16)
    winb = Buf("win")
    for k in range(8):
        S.op("pool", lambda k=k: nc.gpsimd.dma_start(out=win[:, k, :], in_=I.w_in[l][k * 128:(k + 1) * 128, :]),
             writes=[winb], dma=True)
    sln = sb("sln", [128, 2, 512]); slnb = Buf("sln")
    S.op("sp", lambda: nc.sync.dma_start(out=sln[:], in_=I.sgu_ln[l]), writes=[slnb], dma=True)
    swT32 = sb("swT32", [128, 4, 128]); swT = sb("swT", [128, 4, 128], BF16); swTb = Buf("swT")
    S.op("sp", lambda: nc.sync.dma_start(out=swT32[:], in_=I.sgu_wT[l]), writes=[swTb], dma=True)
    S.op("dve", lambda: nc.vector.tensor_copy(swT[:], swT32[:]), reads=[swTb], writes=[swTb])
    sbias = sb("sbias", [128, 4, 128]); sbiasb = Buf("sbias")
    S.op("sp", lambda: nc.sync.dma_start(out=sbias[:], in_=I.sgu_b[l]), writes=[sbiasb], dma=True)
    gpar = sb("gpar", [128, 2, 8]); gparb = Buf("gpar")
    S.op("sp", lambda: nc.sync.dma_start(out=gpar[:], in_=I.gate_par[l]), writes=[gparb], dma=True)
    S.op("act", lambda: nc.scalar.activation(out=gpar[:, 0, :], in_=gpar[:, 0, :], func=AF.Exp), reads=[gparb], writes=[gparb])
    S.op("dve", lambda: nc.vector.tensor_scalar_mul(out=gpar[:, 0, :], in0=gpar[:, 0, :], scalar1=-1.0), reads=[gparb], writes=[gparb])

    NB = 2
    xs = [sb("xs%d" % i, [128, 4, D]) for i in range(NB)]; xsb = [Buf("xs%d" % i) for i in range(NB)]
    hT = [sb("hT%d" % i, [128, 8, 512], BF16) for i in range(NB)]; hTb = [Buf("hT%d" % i) for i in range(NB)]
    xn = sb("xn", [128, D]); xnb = Buf("xn")
    st6 = sb("st6", [128, 3, 6]); stb = Buf("st6")
    uT = sb("uT", [128, 4, 512], BF16); uTb = Buf("uT")
    oaT = [sb("oaT%d" % i, [128, 4, 512], BF16) for i in range(NB)]; oaTb = [Buf("oaT%d" % i) for i in range(NB)]
    stg = [sb("stg%d" % i, [128, 512]) for i in range(4)]; stgb = [Buf("stg%d" % i) for i in range(4)]
    sgs = [sb("sgs%d" % i, [128, 512], BF16) for i in range(4)]; sgsb = [Buf("sgs%d" % i) for i in range(4)]
    vg = sb("vg", [128, 512]); vgb = Buf("vg")
    vn = sb("vn", [128, 512], BF16); vnb = Buf("vn")
    zst = [sb("zst%d" % i, [128, 512]) for i in range(2)]; zstb = [Buf("zst%d" % i) for i in range(2)]
    gt = sb("gt", [128, 4, 16]); gtb = Buf("gt")
    gw = sb("gw", [128, 4, 8]); gwb = Buf("gw")
    mx = sb("mx", [128, 512]); mxb = Buf("mx")

    qkvb = Buf("s_qkvT"); C.b_qkvT = qkvb
    C.b_oaT = Buf("s_oaT"); C.b_sgT = Buf("s_sgT"); C.b_zs = Buf("s_zs"); C.b_glb = Buf("s_glb")
    C.b_x1 = getattr(C, "b_x1", Buf("s_x1"))
    xsrcb = C.b_x1 if l == 1 else Buf("xin")
    own_hi = TC + C.OWN

    col_u, col_v, col_qkv, col_z, col_g, col_ga, col_gb = 0, 512, 1024, 2560, 3072, 3088, 4112
    rot = 0
    stc = 0
    for gi, (r0, nt, isctx) in enumerate(groups_of(C, 4)):
        N = nt * 128
        full = (l == 0) or (not isctx and r0 < own_hi)
        b = gi % NB
        S.op("sp", lambda b=b, r0=r0, nt=nt: nc.sync.dma_start(
            out=xs[b][:, 0:nt, :], in_=xsrc[r0:r0 + nt * 128, :].rearrange("(t p) d -> p t d", p=128)),
            reads=[xsrcb], writes=[xsb[b]], dma=True)
        ln_modT(C, l, xs[b], xsb[b], nt, 0, isctx, hT[b], hTb[b], "a", xn, xnb, st6, stb, use_act=False)
        fm = []
        if full:
            fm += [("u", c, col_u + c * 128) for c in range(4)]
        fm += [("qkv", c, col_qkv + c * 128) for c in range(12)]
        if full:
            fm += [("ga", c, col_ga + c * 128) for c in range(8)] + [("gb", c, col_gb + c * 128) for c in range(8)]
        for (kind, c, col) in fm:
            pb = rot % 4
            rot += 1
            for k in range(8):
                S.op("pe", lambda k=k, col=col, pb=pb, b=b, N=N: nc.tensor.matmul(
                    C.ps[pb][:, 0:N], win[:, k, col:col + 128], hT[b][:, k, 0:N], start=(k == 0), stop=(k == 7)),
                    reads=[winb, hTb[b]], writes=[C.psb[pb]])
            if kind == "u":
                S.op("act", lambda c=c, pb=pb, N=N: nc.scalar.activation(out=uT[:, c, 0:N], in_=C.ps[pb][:, 0:N], func=AF.Gelu),
                     reads=[C.psb[pb]], writes=[uTb])
            elif kind == "qkv":
                si = stc % 4
                stc += 1
                if c % 2 == 0:
                    S.op("dve", lambda si=si, pb=pb, N=N: nc.vector.tensor_copy(stg[si][:, 0:N], C.ps[pb][:, 0:N]),
                         reads=[C.psb[pb]], writes=[stgb[si]])
                else:
                    S.op("dve", lambda si=si, pb=pb, N=N: nc.vector.tensor_copy(out=stg[si][:, 0:N], in_=C.ps[pb][:, 0:N]),
                         reads=[C.psb[pb]], writes=[stgb[si]])
                S.op("sp", lambda si=si, c=c, r0=r0, N=N: nc.sync.dma_start(out=Sx.qkvT[c, :, r0:r0 + N], in_=stg[si][:, 0:N]),
                     reads=[stgb[si]], writes=[qkvb], dma=True)
            else:
                si = stc % 4
                stc += 1
                cc = c + (8 if kind == "gb" else 0)
                S.op("act", lambda si=si, pb=pb, N=N: nc.scalar.activation(out=sgs[si][:, 0:N], in_=C.ps[pb][:, 0:N], func=AF.Sigmoid),
                     reads=[C.psb[pb]], writes=[sgsb[si]])
                S.op("sp", lambda si=si, cc=cc, r0=r0, N=N: nc.sync.dma_start(out=Sx.sgT[cc, :, r0:r0 + N], in_=sgs[si][:, 0:N]),
                     reads=[sgsb[si]], writes=[C.b_sgT], dma=True)
        for t in range(nt):
            tl = slice(t * 128, (t + 1) * 128)
            pb = 4
            for k in range(8):
                S.op("pe", lambda k=k, b=b, tl=tl: nc.tensor.matmul(
                    C.ps[4][:, 0:16], hT[b][:, k, tl], win[:, k, col_g:col_g + 16], start=(k == 0), stop=(k == 7)),
                    reads=[winb, hTb[b]], writes=[C.psb[4]])
            S.op("dve", lambda t=t: nc.vector.tensor_copy(gt[:, t, :], C.ps[4][:, 0:16]), reads=[C.psb[4]], writes=[gtb])
            if full:
                for k in range(8):
                    S.op("pe", lambda k=k, b=b, tl=tl: nc.tensor.matmul(
                        C.ps[5][:, :], hT[b][:, k, tl], win[:, k, col_v:col_v + 512], start=(k == 0), stop=(k == 7)),
                        reads=[winb, hTb[b]], writes=[C.psb[5]])
                S.op("act", lambda: nc.scalar.activation(out=vg[:], in_=C.ps[5][:, :], func=AF.Gelu), reads=[C.psb[5]], writes=[vgb])
                S.op("dve", lambda: nc.vector.bn_stats(out=st6[:, 0, :], in_=vg[:]), reads=[vgb], writes=[stb])
                S.op("dve", lambda: nc.vector.bn_aggr(out=st6[:, 2, 0:2], in_=st6[:, 0:1, :]), reads=[stb], writes=[stb])
                rstd_op(C, st6[:, 2, 2:3], st6[:, 2, 1:2], stb)
                S.op("dve", lambda: nc.vector.tensor_scalar(out=vg[:], in0=vg[:], scalar1=st6[:, 2, 0:1], scalar2=st6[:, 2, 2:3],
                                                            op0=ALU.subtract, op1=ALU.mult), reads=[vgb, stb], writes=[vgb])
                S.op("dve", lambda: nc.vector.tensor_tensor(out=vg[:], in0=vg[:], in1=sln[:, 0, :], op=ALU.mult),
                     reads=[vgb, slnb], writes=[vgb])
                S.op("dve", lambda: nc.vector.tensor_tensor(out=vn[:], in0=vg[:], in1=sln[:, 1, :], op=ALU.add),
                     reads=[vgb, slnb], writes=[vnb])
                for g in range(4):
                    S.op("pe", lambda g=g: nc.tensor.matmul(C.ps[5][:, g * 128:(g + 1) * 128], vn[:, g * 128:(g + 1) * 128],
                                                            swT[:, g, :], start=True, stop=True),
                         reads=[vnb, swTb], writes=[C.psb[5]])
                S.op("dve", lambda: nc.vector.tensor_tensor(out=mx[:], in0=C.ps[5][:, :], in1=sbias[:].rearrange("p g q -> p (g q)"),
                                                            op=ALU.add), reads=[C.psb[5], sbiasb], writes=[mxb])
                S.op("dve", lambda b=b, tl=tl: nc.vector.tensor_tensor(
                    out=oaT[b][:, :, tl], in0=mx[:].rearrange("p (g q) -> p g q", g=4), in1=uT[:, :, tl], op=ALU.mult),
                    reads=[mxb, uTb], writes=[oaTb[b]])
                zi = t % 2
                for k in range(8):
                    S.op("pe", lambda k=k, b=b, tl=tl: nc.tensor.matmul(
                        C.ps[5][:, :], hT[b][:, k, tl], win[:, k, col_z:col_z + 512], start=(k == 0), stop=(k == 7)),
                        reads=[winb, hTb[b]], writes=[C.psb[5]])
                S.op("act", lambda zi=zi: nc.scalar.activation(out=zst[zi][:], in_=C.ps[5][:, :], func=AF.Silu),
                     reads=[C.psb[5]], writes=[zstb[zi]])
                S.op("sp", lambda zi=zi, r0=r0, t=t: nc.sync.dma_start(out=Sx.zs[r0 + t * 128:r0 + (t + 1) * 128, :], in_=zst[zi][:]),
                     reads=[zstb[zi]], writes=[C.b_zs], dma=True)
        if full:
            S.op("sp", lambda b=b, r0=r0, N=N: nc.sync.dma_start(
                out=Sx.oaT[:, :, r0:r0 + N].rearrange("c p t -> p c t"), in_=oaT[b][:, :, 0:N]),
                reads=[oaTb[b]], writes=[C.b_oaT], dma=True)
        gv = gt[:, 0:nt, :].rearrange("p t (d k h) -> p t d k h", d=2, k=2)
        a_v = gv[:, :, :, 0, :]
        b_v = gv[:, :, :, 1, :]
        gwv = gw[:, 0:nt, :].rearrange("p t (d h) -> p t d h", d=2)
        dtb = bc(gpar[:, 1:2, :], [128, nt, 8]).rearrange("p t (d h) -> p t d h", d=2)
        nea = bc(gpar[:, 0:1, :], [128, nt, 8]).rearrange("p t (d h) -> p t d h", d=2)
        S.op("dve", lambda: nc.vector.tensor_tensor(out=a_v, in0=a_v, in1=dtb, op=ALU.add), reads=[gtb, gparb], writes=[gtb])
        S.op("act", lambda: nc.scalar.activation(out=gwv, in_=a_v, func=AF.Abs), reads=[gtb], writes=[gwb])
        S.op("act", lambda: nc.scalar.activation(out=gw[:, 0:nt, :], in_=gw[:, 0:nt, :], func=AF.Exp, scale=-1.0), reads=[gwb], writes=[gwb])
        S.op("act", lambda: nc.scalar.activation(out=gw[:, 0:nt, :], in_=gw[:, 0:nt, :], func=AF.Ln, bias=1.0), reads=[gwb], writes=[gwb])
        S.op("dve", lambda: nc.vector.tensor_scalar_max(out=a_v, in0=a_v, scalar1=0.0), reads=[gtb], writes=[gtb])
        S.op("dve", lambda: nc.vector.tensor_tensor(out=a_v, in0=a_v, in1=gwv, op=ALU.add), reads=[gtb, gwb], writes=[gtb])
        S.op("dve", lambda: nc.vector.tensor_tensor(out=a_v, in0=a_v, in1=nea, op=ALU.mult), reads=[gtb, gparb], writes=[gtb])
        S.op("act", lambda: nc.scalar.activation(out=b_v, in_=b_v, func=AF.Sigmoid), reads=[gtb], writes=[gtb])
        S.op("sp", lambda r0=r0, nt=nt: nc.sync.dma_start(
            out=Sx.glb[r0:r0 + nt * 128, :].rearrange("(t p) c -> p t c", p=128), in_=gt[:, 0:nt, :]),
            reads=[gtb], writes=[C.b_glb], dma=True)


def pass_b(C, l):
    nc, S, I, Sx, ls = C.nc, C.S, C.I, C.Sx, C.ls
    T, TL = C.T, C.TL
    sb = lambda name, shape, dt=F32: ls.enter_context(nc.sbuf_tensor(name + "_b%d" % l, list(shape), dt))
    PL = min(2048, TL)
    cw = sb("cw", [128, 12, 5]); cwb = Buf("cw")
    S.op("sp", lambda: nc.sync.dma_start(out=cw[:], in_=I.conv_w[l]), writes=[cwb], dma=True)
    NB = 2
    xin = [sb("xin%d" % i, [128, PL + 4]) for i in range(NB)]; xinb = [Buf("xin%d" % i) for i in range(NB)]
    acc = [sb("acc%d" % i, [128, PL]) for i in range(NB)]; accb = [Buf("acc%d" % i) for i in range(NB)]
    yv = [sb("yv%d" % i, [128, PL]) for i in range(NB)]; yvb = [Buf("yv%d" % i) for i in range(NB)]
    sq = sb("sq", [128, PL]); sqb = Buf("sq")
    rs = [sb("rs%d" % i, [128, 512]) for i in range(2)]; rsb = [Buf("rs%d" % i) for i in range(2)]
    tk = [sb("tk%d" % i, [128, 4, 128]) for i in range(2)]; tkb = [Buf("tk%d" % i) for i in range(2)]
    C.b_qkn = Buf("s_qkn"); C.b_ktok = Buf("s_ktok"); C.b_vtok = Buf("s_vtok")
    segs = [(0, TC, 0, TC)]
    r = TC
    while r < T:
        segs.append((r, min(PL, T - r), TC, T))
        r += PL
    it = 0
    rsi = 0
    tki = 0
    for j in range(12):
        for (r0, L, s0, s1) in segs:
            b = it % NB
            it += 1
            lo = max(s0, r0 - 2)
            hi = min(s1, r0 + L + 2)
            if lo > r0 - 2:
                S.op("dve", lambda b=b: nc.vector.memset(xin[b][:, 0:2], 0.0), writes=[xinb[b]])
            if hi < r0 + L + 2:
                S.op("dve", lambda b=b, L=L: nc.vector.memset(xin[b][:, L + 2:L + 4], 0.0), writes=[xinb[b]])
            S.op("sp", lambda b=b, lo=lo, hi=hi, r0=r0: nc.sync.dma_start(
                out=xin[b][:, lo - (r0 - 2):hi - (r0 - 2)], in_=Sx.qkvT[j, :, lo:hi]),
                reads=[C.b_qkvT], writes=[xinb[b]], dma=True)
            S.op("dve", lambda b=b, L=L: nc.vector.tensor_scalar_mul(out=acc[b][:, 0:L], in0=xin[b][:, 0:L], scalar1=cw[:, j, 0:1]),
                 reads=[xinb[b], cwb], writes=[accb[b]])
            for tap in range(1, 5):
                S.op("dve", lambda b=b, L=L, tap=tap: nc.vector.scalar_tensor_tensor(
                    out=acc[b][:, 0:L], in0=xin[b][:, tap:tap + L], scalar=cw[:, j, tap:tap + 1], in1=acc[b][:, 0:L],
                    op0=ALU.mult, op1=ALU.add), reads=[xinb[b], cwb, accb[b]], writes=[accb[b]])
            S.op("act", lambda b=b, L=L: nc.scalar.activation(out=yv[b][:, 0:L], in_=acc[b][:, 0:L], func=AF.Silu),
                 reads=[accb[b]], writes=[yvb[b]])
            if j < 8:
                S.op("act", lambda b=b, L=L: nc.scalar.activation(out=sq[:, 0:L], in_=yv[b][:, 0:L], func=AF.Square),
                     reads=[yvb[b]], writes=[sqb])
                for c0 in range(0, L, 512):
                    n = min(512, L - c0)
                    pb = 0 + (c0 // 512) % 2
                    ri = rsi % 2
                    rsi += 1
                    S.op("pe", lambda c0=c0, n=n, pb=pb: nc.tensor.matmul(C.ps[pb][:, 0:n], C.cst[:, 1, :], sq[:, c0:c0 + n],
                                                                          start=True, stop=True),
                         reads=[sqb, C.cstb], writes=[C.psb[pb]])
                    S.op("act", lambda n=n, pb=pb, ri=ri: nc.scalar.activation(
                        out=rs[ri][:, 0:n], in_=C.ps[pb][:, 0:n], func=AF.Sqrt, bias=C.epsc[:, 0:1], scale=1.0),
                        reads=[C.psb[pb], C.cstb], writes=[rsb[ri]])
                    S.op("dve", lambda n=n, ri=ri: nc.vector.reciprocal(out=rs[ri][:, 0:n], in_=rs[ri][:, 0:n]),
                         reads=[rsb[ri]], writes=[rsb[ri]])
                    qs = (128.0 ** -0.5) if j < 4 else 1.0
                    S.op("dve", lambda b=b, c0=c0, n=n, ri=ri, qs=qs: nc.vector.scalar_tensor_tensor(
                        out=yv[b][:, c0:c0 + n], in0=yv[b][:, c0:c0 + n], scalar=qs, in1=rs[ri][:, 0:n],
                        op0=ALU.mult, op1=ALU.mult), reads=[yvb[b], rsb[ri]], writes=[yvb[b]])
                S.op("sp", lambda b=b, L=L, r0=r0: nc.sync.dma_start(out=Sx.qkn[j, :, r0:r0 + L], in_=yv[b][:, 0:L]),
                     reads=[yvb[b]], writes=[C.b_qkn], dma=True)
            if j >= 4:
                dst, dstb = (Sx.ktok, C.b_ktok) if j < 8 else (Sx.vtok, C.b_vtok)
                h = j % 4
                for c0 in range(0, L, 512):
                    n = min(512, L - c0)
                    nt = n // 128
                    pb = 2 + (c0 // 512) % 2
                    ti = tki % 2
                    tki += 1
                    for t in range(nt):
                        S.op("pe", lambda b=b, c0=c0, t=t, pb=pb: nc.tensor.transpose(
                            C.ps[pb][:, t * 128:(t + 1) * 128], yv[b][:, c0 + t * 128:c0 + (t + 1) * 128], C.cst[:, 0, :]),
                            reads=[yvb[b], C.cstb], writes=[C.psb[pb]])
                    S.op("dve", lambda ti=ti, pb=pb, n=n: nc.vector.tensor_copy(
                        out=tk[ti][:].rearrange("p t d -> p (t d)")[:, 0:n], in_=C.ps[pb][:, 0:n]),
                        reads=[C.psb[pb]], writes=[tkb[ti]])
                    S.op("sp", lambda ti=ti, r0=r0, c0=c0, nt=nt, n=n, h=h, dst=dst: nc.sync.dma_start(
                        out=dst[r0 + c0:r0 + c0 + n, h * 128:(h + 1) * 128].rearrange("(t p) d -> p t d", p=128),
                        in_=tk[ti][:, 0:nt, :]), reads=[tkb[ti]], writes=[dstb], dma=True)


def pass_cd(C, l):
    nc, S, I, Sx, ls = C.nc, C.S, C.I, C.Sx, C.ls
    T, NCH = C.T, C.NCH
    sb = lambda name, shape, dt=F32: ls.enter_context(nc.sbuf_tensor(name + "_c%d" % l, list(shape), dt))
    DT = F32
    cst = C.cst
    ident64 = cst[0:64, 0, 0:64]
    ones64 = cst[0:64, 1, :]
    tri = [cst[0:64, 2, 0:64], cst[0:64, 3, 0:64]]
    maskI = [cst[0:64, 2, 0:64], cst[0:64, 3, 0:64]]
    maskS = [cst[0:64, 4, 0:64], cst[0:64, 5, 0:64]]
    NB = 2
    qk = [[sb("qk%d_%d" % (d, i), [128, 8, 256]) for i in range(NB)] for d in range(2)]
    qkb = [[Buf("qk%d_%d" % (d, i)) for i in range(NB)] for d in range(2)]
    kt = [[sb("kt%d_%d" % (d, i), [64, 4, 512]) for i in range(NB)] for d in range(2)]
    ktb = [[Buf("kt%d_%d" % (d, i)) for i in range(NB)] for d in range(2)]
    vt = [[sb("vt%d_%d" % (d, i), [64, 4, 512]) for i in range(NB)] for d in range(2)]
    vtb = [[Buf("vt%d_%d" % (d, i)) for i in range(NB)] for d in range(2)]
    gl = [[sb("gl%d_%d" % (d, i), [64, 4, 16]) for i in range(NB)] for d in range(2)]
    glb_ = [[Buf("gl%d_%d" % (d, i)) for i in range(NB)] for d in range(2)]
    St = sb("St", [128, 8, 128]); Stb = Buf("St")
    S.op("dve", lambda: nc.vector.memset(St[:], 0.0), writes=[Stb])
    lgB = sb("lgB", [64, 8, 128]); lgBb = Buf("lgB")
    gcol = sb("gcol", [64, 8]); gcolb = Buf("gcol")
    gcB = sb("gcB", [128, 8]); gcBb = Buf("gcB")
    e1 = sb("e1", [64, 8]); e1b = Buf("e1")
    egc = sb("egc", [64, 8]); egcb = Buf("egc")
    bet = sb("bet", [64, 8]); betb = Buf("bet")
    dmat = sb("dmat", [64, 8, 64]); dmatb = Buf("dmat")
    E12 = sb("E12", [64, 2, 8, 64]); E12b = Buf("E12")
    E12m = sb("E12m", [64, 2, 8, 64]); E12mb = Buf("E12m")
    expG = sb("expG", [128, 8, 64]); expGb = Buf("expG")
    qg = sb("qg", [128, 8, 64], DT); qgb = Buf("qg")
    kd = sb("kd", [64, 8, 128], DT); kdb = Buf("kd")
    bek = sb("bek", [64, 8, 128]); bekb = Buf("bek")
    bv = sb("bv", [64, 8, 128]); bvb = Buf("bv")
    Lp = [sb("Lp%d" % i, [64, 8, 64]) for i in range(2)]; Lpb = [Buf("Lp%d" % i) for i in range(2)]
    Ap = [sb("Ap%d" % i, [64, 8, 64]) for i in range(2)]; Apb = [Buf("Ap%d" % i) for i in range(2)]
    qkm = sb("qkm", [64, 8, 64], DT); qkmb = Buf("qkm")
    Pm = sb("Pm", [64, 8, 64]); Pmb = Buf("Pm")
    usb = sb("usb", [64, 8, 128]); usbb = Buf("usb")
    wkT = sb("wkT", [128, 8, 64], DT); wkTb = Buf("wkT")
    Wsb = sb("Wsb", [64, 8, 128], DT); Wsbb = Buf("Wsb")
    osb = [sb("osb%d" % i, [64, 8, 128]) for i in range(4)]; osbb = [Buf("osb%d" % i) for i in range(4)]
    C.b_of = Buf("s_of"); C.b_ob = Buf("s_ob")
    E2s = sb("E2s", [64, 8, 64]); E2sb = Buf("E2s")
    dbet = sb("dbet", [64, 8, 64]); dbetb = Buf("dbet")
    identB = sb("identB", [64, 8, 64]); identBb = Buf("identB")
    for blk in range(8):
        S.op("dve", lambda blk=blk: nc.vector.tensor_copy(identB[:, blk, :], ident64), reads=[C.cstb], writes=[identBb])
    ps, psb = C.ps, C.psb
    P64 = lambda bank: ps[bank][0:64, :].rearrange("p (b c) -> p b c", b=8)

    import os
    lim = int(os.environ.get("CD_STAGE", "99"))
    ngroups = min(NCH // 4, int(os.environ.get("CD_GROUPS", "9999")))
    only = os.environ.get("CD_ONLY")
    for gI in range(ngroups):
        if only and str(gI) not in only.split(","):
            continue
        b = gI % NB
        r1 = gI * 256
        ct = (3 if gI == 0 else NCH + 3 - 4 * gI)
        r2 = (ct - 3) * 64
        for d, r in ((0, r1), (1, r2)):
            S.op("sp", lambda d=d, r=r, b=b: nc.sync.dma_start(
                out=qk[d][b][:], in_=Sx.qkn[:, :, r:r + 256].rearrange("j p t -> p j t")),
                reads=[C.b_qkn], writes=[qkb[d][b]], dma=True)
            S.op("sp", lambda d=d, r=r, b=b: nc.sync.dma_start(
                out=kt[d][b][:], in_=Sx.ktok[r:r + 256, :].rearrange("(n p) f -> p n f", p=64)),
                reads=[C.b_ktok], writes=[ktb[d][b]], dma=True)
            S.op("sp", lambda d=d, r=r, b=b: nc.sync.dma_start(
                out=vt[d][b][:], in_=Sx.vtok[r:r + 256, :].rearrange("(n p) f -> p n f", p=64)),
                reads=[C.b_vtok], writes=[vtb[d][b]], dma=True)
            S.op("sp", lambda d=d, r=r, b=b: nc.sync.dma_start(
                out=gl[d][b][:], in_=Sx.glb[r:r + 256, :].rearrange("(n p) f -> p n f", p=64)),
                reads=[C.b_glb], writes=[glb_[d][b]], dma=True)
        for si in range(min(4, int(os.environ.get("CD_STEPS", "4")))):
            ci = [si, 3 - si]
            rows = [r1 + si * 64, r2 + (3 - si) * 64]
            lgv = [gl[d][b][:, ci[d], d * 8:d * 8 + 4] for d in range(2)]
            btv = [gl[d][b][:, ci[d], d * 8 + 4:d * 8 + 8] for d in range(2)]
            qT = [[qk[d][b][:, h, ci[d] * 64:(ci[d] + 1) * 64] for h in range(4)] for d in range(2)]
            kT = [[qk[d][b][:, 4 + h, ci[d] * 64:(ci[d] + 1) * 64] for h in range(4)] for d in range(2)]
            ktv = [kt[d][b][:, ci[d], :].rearrange("p (h e) -> p h e", h=4) for d in range(2)]
            vtv = [vt[d][b][:, ci[d], :].rearrange("p (h e) -> p h e", h=4) for d in range(2)]
            for d in range(2):
                S.op("dve", lambda d=d: nc.vector.tensor_copy(lgB[:, d * 4:(d + 1) * 4, :], bc(lgv[d].unsqueeze(2), [64, 4, 128])),
                     reads=[glb_[d][b]], writes=[lgBb])
                S.op("dve", lambda d=d: nc.vector.tensor_copy(bet[:, d * 4:(d + 1) * 4], btv[d]), reads=[glb_[d][b]], writes=[betb])
            for d in range(2):
                for h in range(4):
                    blk = d * 4 + h
                    S.op("pe", lambda d=d, blk=blk: nc.tensor.matmul(ps[0][:, blk * 64:(blk + 1) * 64], lgB[:, blk, :], tri[d],
                                                                     start=True, stop=True),
                         reads=[lgBb, C.cstb], writes=[psb[0]])
                S.op("pe", lambda d=d: nc.tensor.matmul(ps[1][0:64, d * 16:d * 16 + 4], tri[d], lgv[d], start=True, stop=True),
                     reads=[glb_[d][b], C.cstb], writes=[psb[1]])
                S.op("pe", lambda d=d: nc.tensor.matmul(ps[1][:, d * 16 + 8:d * 16 + 12], ones64, lgv[d], start=True, stop=True),
                     reads=[glb_[d][b], C.cstb], writes=[psb[1]])
            p1v = ps[1][:, 0:32].rearrange("p (d x) -> p d x", d=2)
            if lim < 3:
                continue
            S.op("dve", lambda: nc.vector.tensor_copy(gcol[:].rearrange("p (d h) -> p d h", d=2), p1v[0:64, :, 0:4]),
                 reads=[psb[1]], writes=[gcolb])
            S.op("act", lambda: nc.scalar.activation(out=gcB[:].rearrange("p (d h) -> p d h", d=2), in_=p1v[:, :, 8:12], func=AF.Exp),
                 reads=[psb[1]], writes=[gcBb])
            S.op("dve", lambda: nc.vector.tensor_tensor(out=e1[:].rearrange("p (d h) -> p d h", d=2), in0=p1v[0:64, :, 8:12],
                                                        in1=gcol[:].rearrange("p (d h) -> p d h", d=2), op=ALU.subtract),
                 reads=[psb[1], gcolb], writes=[e1b])
            S.op("act", lambda: nc.scalar.activation(out=e1[:], in_=e1[:], func=AF.Exp), reads=[e1b], writes=[e1b])
            S.op("act", lambda: nc.scalar.activation(out=egc[:], in_=gcol[:], func=AF.Exp), reads=[gcolb], writes=[egcb])
            S.op("dve", lambda: nc.vector.tensor_tensor(out=egc[:], in0=egc[:], in1=bet[:], op=ALU.mult),
                 reads=[egcb, betb], writes=[egcb])
            if lim < 4:
                continue
            S.op("dve", lambda: nc.vector.tensor_tensor(out=dmat[:], in0=P64(0), in1=bc(gcol[:].unsqueeze(2), [64, 8, 64]),
                                                        op=ALU.subtract), reads=[psb[0], gcolb], writes=[dmatb])
            S.op("dve", lambda: nc.vector.tensor_scalar(out=E12[:, 0, :, :], in0=dmat[:], scalar1=-1.0, scalar2=0.0,
                                                        op0=ALU.mult, op1=ALU.min), reads=[dmatb], writes=[E12b])
            S.op("dve", lambda: nc.vector.tensor_scalar_min(out=E12[:, 1, :, :], in0=dmat[:], scalar1=0.0),
                 reads=[dmatb], writes=[E12b])
            S.op("act", lambda: nc.scalar.activation(out=E12[:], in_=E12[:], func=AF.Exp), reads=[E12b], writes=[E12b])
            S.op("act", lambda: nc.scalar.activation(out=expG[:], in_=ps[0][:, :].rearrange("p (b c) -> p b c", b=8), func=AF.Exp),
                 reads=[psb[0]], writes=[expGb])
            for d in range(2):
                S.op("pool", lambda d=d: nc.gpsimd.tensor_tensor(
                    out=E12m[:, 0, d * 4:(d + 1) * 4, :], in0=E12[:, 0, d * 4:(d + 1) * 4, :],
                    in1=bc(maskS[d].unsqueeze(1), [64, 4, 64]), op=ALU.mult), reads=[E12b, C.cstb], writes=[E12mb])
                S.op("pool", lambda d=d: nc.gpsimd.tensor_tensor(
                    out=E12m[:, 1, d * 4:(d + 1) * 4, :], in0=E12[:, 1, d * 4:(d + 1) * 4, :],
                    in1=bc(maskI[d].unsqueeze(1), [64, 4, 64]), op=ALU.mult), reads=[E12b, C.cstb], writes=[E12mb])
                S.op("pool", lambda d=d: nc.gpsimd.tensor_tensor(
                    out=E2s[:, d * 4:(d + 1) * 4, :], in0=E12[:, 1, d * 4:(d + 1) * 4, :],
                    in1=bc(maskS[1 - d].unsqueeze(1), [64, 4, 64]), op=ALU.mult), reads=[E12b, C.cstb], writes=[E2sb])
                S.op("dve", lambda d=d: nc.vector.tensor_tensor(
                    out=qg[:, d * 4:(d + 1) * 4, :], in0=qk[d][b][:, 0:4, ci[d] * 64:(ci[d] + 1) * 64],
                    in1=expG[:, d * 4:(d + 1) * 4, :], op=ALU.mult), reads=[qkb[d][b], expGb], writes=[qgb])
                S.op("dve", lambda d=d: nc.vector.tensor_tensor(
                    out=kd[:, d * 4:(d + 1) * 4, :], in0=ktv[d], in1=bc(e1[:, d * 4:(d + 1) * 4].unsqueeze(2), [64, 4, 128]),
                    op=ALU.mult), reads=[ktb[d][b], e1b], writes=[kdb])
                S.op("pool", lambda d=d: nc.gpsimd.tensor_tensor(
                    out=bek[:, d * 4:(d + 1) * 4, :], in0=ktv[d], in1=bc(egc[:, d * 4:(d + 1) * 4].unsqueeze(2), [64, 4, 128]),
                    op=ALU.mult), reads=[ktb[d][b], egcb], writes=[bekb])
                S.op("pool", lambda d=d: nc.gpsimd.tensor_tensor(
                    out=bv[:, d * 4:(d + 1) * 4, :], in0=vtv[d], in1=bc(bet[:, d * 4:(d + 1) * 4].unsqueeze(2), [64, 4, 128]),
                    op=ALU.mult), reads=[vtb[d][b], betb], writes=[bvb])
            if lim < 6:
                continue
            for d in range(2):
                for h in range(4):
                    blk = d * 4 + h
                    S.op("pe", lambda d=d, h=h, blk=blk: nc.tensor.matmul(ps[2][0:64, blk * 64:(blk + 1) * 64], kT[d][h], kT[d][h],
                                                                          start=True, stop=True),
                         reads=[qkb[d][b]], writes=[psb[2]])
                    S.op("pe", lambda d=d, h=h, blk=blk: nc.tensor.matmul(ps[3][0:64, blk * 64:(blk + 1) * 64], kT[d][h], qT[d][h],
                                                                          start=True, stop=True),
                         reads=[qkb[d][b]], writes=[psb[3]])
            if lim < 7:
                continue
            S.op("dve", lambda: nc.vector.tensor_tensor(out=Lp[0][:], in0=P64(2), in1=E12m[:, 0, :, :], op=ALU.mult),
                 reads=[psb[2], E12mb], writes=[Lpb[0]])
            S.op("dve", lambda: nc.vector.tensor_tensor(out=Lp[0][:], in0=Lp[0][:], in1=bc(bet[:].unsqueeze(2), [64, 8, 64]),
                                                        op=ALU.mult), reads=[Lpb[0], betb], writes=[Lpb[0]])
            S.op("dve", lambda: nc.vector.tensor_tensor(out=qkm[:], in0=P64(3), in1=E12m[:, 1, :, :], op=ALU.mult),
                 reads=[psb[3], E12mb], writes=[qkmb])
            if lim < 8:
                continue
            S.op("dve", lambda: nc.vector.tensor_tensor(out=dbet[:], in0=identB[:], in1=bc(bet[:].unsqueeze(2), [64, 8, 64]),
                                                        op=ALU.mult), reads=[betb, identBb], writes=[dbetb])
            for blk in range(8):
                S.op("pe", lambda blk=blk: nc.tensor.matmul(ps[7][0:64, blk * 64:(blk + 1) * 64], cst[0:64, 1, 0:64], dbet[:, blk, :],
                                                            start=True, stop=True), reads=[dbetb, C.cstb], writes=[psb[7]])
            S.op("dve", lambda: nc.vector.tensor_tensor(out=Ap[0][:], in0=P64(2), in1=E2s[:], op=ALU.mult),
                 reads=[psb[2], E2sb], writes=[Apb[0]])
            S.op("dve", lambda: nc.vector.tensor_tensor(out=Ap[0][:], in0=Ap[0][:], in1=P64(7), op=ALU.mult),
                 reads=[psb[7], Apb[0]], writes=[Apb[0]])
            S.op("pool", lambda: nc.gpsimd.tensor_tensor(out=Pm[:], in0=identB[:], in1=Ap[0][:], op=ALU.subtract),
                 reads=[Apb[0], identBb], writes=[Pmb])
            if lim < 9:
                continue
            for j in range(1, 6):
                cur, prv = j % 2, (j - 1) % 2
                for blk in range(8):
                    S.op("pe", lambda blk=blk, prv=prv: nc.tensor.matmul(ps[5][0:64, blk * 64:(blk + 1) * 64], Ap[prv][:, blk, :],
                                                                         Lp[prv][:, blk, :], start=True, stop=True),
                         reads=[Apb[prv], Lpb[prv]], writes=[psb[5]])
                if j <= 4:
                    for blk in range(8):
                        S.op("pe", lambda blk=blk, prv=prv: nc.tensor.matmul(ps[6][0:64, blk * 64:(blk + 1) * 64], Lp[prv][:, blk, :],
                                                                             Ap[prv][:, blk, :], start=True, stop=True),
                             reads=[Apb[prv], Lpb[prv]], writes=[psb[6]])
                S.op("dve", lambda cur=cur: nc.vector.tensor_copy(out=Lp[cur][:], in_=P64(5)), reads=[psb[5]], writes=[Lpb[cur]])
                if j <= 4:
                    S.op("dve", lambda cur=cur: nc.vector.tensor_copy(Ap[cur][:], P64(6)), reads=[psb[6]], writes=[Apb[cur]])
                for blk in range(8):
                    S.op("pe", lambda blk=blk, cur=cur: nc.tensor.matmul(ps[7][0:64, blk * 64:(blk + 1) * 64], Lp[cur][:, blk, :],
                                                                         Pm[:, blk, :], start=True, stop=True),
                         reads=[Lpb[cur], Pmb], writes=[psb[7]])
                S.op("dve", lambda: nc.vector.tensor_tensor(out=Pm[:], in0=Pm[:], in1=P64(7), op=ALU.add),
                     reads=[Pmb, psb[7]], writes=[Pmb])
            if lim < 10:
                continue
            for d in range(2):
                for h in range(4):
                    blk = d * 4 + h
                    S.op("pe", lambda d=d, h=h, blk=blk: nc.tensor.matmul(ps[2 + d][0:64, h * 128:(h + 1) * 128], Pm[:, blk, :],
                                                                          bv[:, blk, :], start=True, stop=True),
                         reads=[Pmb, bvb], writes=[psb[2 + d]])
                    S.op("pe", lambda blk=blk: nc.tensor.matmul(ps[4][:, blk * 64:(blk + 1) * 64], bek[:, blk, :], Pm[:, blk, :],
                                                                start=True, stop=True),
                         reads=[Pmb, bekb], writes=[psb[4]])
            for d in range(2):
                S.op("dve", lambda d=d: nc.vector.tensor_copy(out=usb[:, d * 4:(d + 1) * 4, :].rearrange("p b e -> p (b e)"),
                                                       in_=ps[2 + d][0:64, :]), reads=[psb[2 + d]], writes=[usbb])
            S.op("dve", lambda: nc.vector.tensor_copy(wkT[:].rearrange("p b c -> p (b c)"), ps[4][:, :]),
                 reads=[psb[4]], writes=[wkTb])
            if lim < 11:
                continue
            for d in range(2):
                for h in range(4):
                    blk = d * 4 + h
                    S.op("pe", lambda d=d, h=h, blk=blk: nc.tensor.matmul(ps[5 + d][0:64, h * 128:(h + 1) * 128], wkT[:, blk, :],
                                                                          St[:, blk, :], start=True, stop=True),
                         reads=[wkTb, Stb], writes=[psb[5 + d]])
            for d in range(2):
                S.op("dve", lambda d=d: nc.vector.tensor_tensor(
                    out=Wsb[:, d * 4:(d + 1) * 4, :].rearrange("p b e -> p (b e)"),
                    in0=usb[:, d * 4:(d + 1) * 4, :].rearrange("p b e -> p (b e)"), in1=ps[5 + d][0:64, :], op=ALU.subtract),
                    reads=[usbb, psb[5 + d]], writes=[Wsbb])
            sub = int(os.environ.get("CD_SUB", "9"))
            if sub < 2:
                continue
            ob_ = (0, 7)
            for d in range(2):
                for h in range(4):
                    blk = d * 4 + h
                    S.op("pe", lambda d=d, h=h, blk=blk: nc.tensor.matmul(ps[ob_[d]][0:64, h * 128:(h + 1) * 128], qg[:, blk, :],
                                                                          St[:, blk, :], start=True, stop=True),
                         reads=[qgb, Stb], writes=[psb[ob_[d]]])
                    S.op("pe", lambda d=d, h=h, blk=blk: nc.tensor.matmul(ps[5 + d][0:64, h * 128:(h + 1) * 128], qkm[:, blk, :],
                                                                          Wsb[:, blk, :], start=True, stop=True),
                         reads=[qkmb, Wsbb], writes=[psb[5 + d]])
            oi = si
            for d in range(2):
                S.op("dve", lambda d=d, oi=oi: nc.vector.tensor_copy(out=osb[oi][:, d * 4:(d + 1) * 4, :].rearrange("p b e -> p (b e)"),
                                                              in_=ps[ob_[d]][0:64, :]), reads=[psb[ob_[d]]], writes=[osbb[oi]])
                S.op("dve", lambda d=d, oi=oi: nc.vector.tensor_tensor(
                    out=osb[oi][:, d * 4:(d + 1) * 4, :].rearrange("p b e -> p (b e)"),
                    in0=osb[oi][:, d * 4:(d + 1) * 4, :].rearrange("p b e -> p (b e)"), in1=ps[5 + d][0:64, :], op=ALU.add),
                    reads=[psb[5 + d], osbb[oi]], writes=[osbb[oi]])
                dst, dstb = (Sx.of, C.b_of) if d == 0 else (Sx.ob, C.b_ob)
                S.op("sp", lambda d=d, oi=oi, dst=dst: nc.sync.dma_start(
                    out=dst[rows[d]:rows[d] + 64, :], in_=osb[oi][:, d * 4:(d + 1) * 4, :].rearrange("p b e -> p (b e)")),
                    reads=[osbb[oi]], writes=[dstb], dma=True)
            if sub < 3:
                continue
            for d in range(2):
                for h in range(4):
                    blk = d * 4 + h
                    S.op("pe", lambda d=d, h=h, blk=blk: nc.tensor.matmul(ps[2 + d][:, h * 128:(h + 1) * 128], kd[:, blk, :],
                                                                          Wsb[:, blk, :], start=True, stop=True),
                         reads=[kdb, Wsbb], writes=[psb[2 + d]])
            if sub < 4:
                continue
            for d in range(2):
                for h in range(4):
                    blk = d * 4 + h
                    S.op("dve", lambda d=d, h=h, blk=blk: nc.vector.scalar_tensor_tensor(
                        out=St[:, blk, :], in0=St[:, blk, :], scalar=gcB[:, blk:blk + 1], in1=ps[2 + d][:, h * 128:(h + 1) * 128],
                        op0=ALU.mult, op1=ALU.add), reads=[Stb, gcBb, psb[2 + d]], writes=[Stb])


def pass_e(C, l):
    nc, S, I, Sx, ls = C.nc, C.S, C.I, C.Sx, C.ls
    T = C.T
    sb = lambda name, shape, dt=F32: ls.enter_context(nc.sbuf_tensor(name + "_e%d" % l, list(shape), dt))
    ps, psb = C.ps, C.psb
    xsrc = I.x if l == 0 else Sx.x1
    xsrcb = Buf("xin_e") if l == 0 else C.b_x1
    wpa = sb("wpa", [128, 4, D], BF16); wpab = Buf("wpa")
    wpb = sb("wpb", [128, 4, D], BF16); wpbb = Buf("wpb")
    wo = sb("wo", [128, 8, D], BF16); wob = Buf("wo")
    S.op("pool", lambda: nc.gpsimd.dma_start(out=wpa[:], in_=I.w_pa[l].rearrange("(c p) d -> p c d", p=128)), writes=[wpab], dma=True)
    S.op("pool", lambda: nc.gpsimd.dma_start(out=wpb[:], in_=I.w_pb[l].rearrange("(c p) d -> p c d", p=128)), writes=[wpbb], dma=True)
    S.op("pool", lambda: nc.gpsimd.dma_start(out=wo[:], in_=I.w_o[l].rearrange("(c p) d -> p c d", p=128)), writes=[wob], dma=True)
    lnp = sb("lnp", [128, 4, D]); lnpb = Buf("lnp")
    S.op("sp", lambda: nc.sync.dma_start(out=lnp[:], in_=I.lnp[l]), writes=[lnpb], dma=True)
    nw = sb("nw", [128, 512]); nwb = Buf("nw")
    S.op("sp", lambda: nc.sync.dma_start(out=nw[:], in_=I.gdn_nw[l]), writes=[nwb], dma=True)
    moe = (l == 1)
    if moe:
        rt = sb("rt", [128, 8, NE]); rtb = Buf("rt")
        S.op("sp", lambda: nc.sync.dma_start(out=rt[:], in_=I.router[:, :, :]), writes=[rtb], dma=True)
        h32 = sb("h32", [128, 8, 512]); h32b = Buf("h32")
        lg8 = sb("lg8", [128, 4, NE]); lg8b = Buf("lg8")
        lgx = sb("lgx", [128, 4, NE]); lgxb = Buf("lgx")
        eq = sb("eq", [128, 2, 4, NE]); eqb = Buf("eq")
        mm = sb("mm", [128, 4, 4]); mmb = Buf("mm")
        gate = sb("gate", [128, 4, NE]); gateb = Buf("gate")
    tA = sb("tA", [128, 512]); tAb = Buf("tA")
    tB = sb("tB", [128, 512]); tBb = Buf("tB")
    tZ = sb("tZ", [128, 512]); tZb = Buf("tZ")
    ss = sb("ss", [128, 8]); ssb = Buf("ss")
    goT = sb("goT", [128, 4, 512], BF16); goTb = Buf("goT")
    oaT = sb("oaT", [128, 4, 512], BF16); oaTb = Buf("oaT")
    sg = [sb("sg%d" % i, [128, 2, 512], BF16) for i in range(2)]; sgb = [Buf("sg%d" % i) for i in range(2)]
    yT = sb("yT", [128, 8, 512], BF16); yTb = Buf("yT")
    xt = sb("xt", [128, D]); xtb = Buf("xt")
    rr = sb("rr", [128, D]); rrb = Buf("rr")
    x1t = sb("x1t", [128, 4, D]); x1tb = Buf("x1t")
    xn = sb("xn", [128, D]); xnb = Buf("xn")
    st6 = sb("st6", [128, 3, 6]); stb = Buf("st6")
    h2T = sb("h2T", [128, 8, 512], BF16); h2Tb = Buf("h2T")
    facc = sb("facc", [128, 4, D]); faccb = Buf("facc")
    NWB = 2
    w1b = [sb("w1b%d" % i, [128, 8, 256], BF16) for i in range(NWB)]
    w3b = [sb("w3b%d" % i, [128, 8, 256], BF16) for i in range(NWB)]
    w2b = [sb("w2b%d" % i, [128, 2, D], BF16) for i in range(NWB)]
    wbb = [Buf("wblk%d" % i) for i in range(NWB)]
    sa = [sb("sa%d" % i, [128, 512], BF16) for i in range(2)]; sab = [Buf("sa%d" % i) for i in range(2)]
    gT = [sb("gT%d" % i, [128, 2, 512], BF16) for i in range(2)]; gTb = [Buf("gT%d" % i) for i in range(2)]
    own_hi = TC + C.OWN
    dstb = C.b_x1 if l == 0 else Buf("yout")
    C.b_x1 = getattr(C, "b_x1", Buf("s_x1"))
    if l == 0:
        dstb = C.b_x1
    wit = 0
    for gi, (r0, nt, isctx) in enumerate(groups_of(C, 4)):
        if l == 1 and (isctx or r0 >= own_hi):
            continue
        N = nt * 128
        for t in range(nt):
            rows = slice(r0 + t * 128, r0 + (t + 1) * 128)
            S.op("sp", lambda rows=rows: nc.sync.dma_start(out=tA[:], in_=Sx.of[rows, :]), reads=[C.b_of], writes=[tAb], dma=True)
            S.op("sp", lambda rows=rows: nc.sync.dma_start(out=tB[:], in_=Sx.ob[rows, :]), reads=[C.b_ob], writes=[tBb], dma=True)
            S.op("sp", lambda rows=rows: nc.sync.dma_start(out=tZ[:], in_=Sx.zs[rows, :]), reads=[C.b_zs], writes=[tZb], dma=True)
            S.op("dve", lambda: nc.vector.tensor_tensor(out=tA[:], in0=tA[:], in1=tB[:], op=ALU.add), reads=[tAb, tBb], writes=[tAb])
            S.op("dve", lambda: nc.vector.tensor_tensor(out=tB[:], in0=tA[:], in1=tA[:], op=ALU.mult), reads=[tAb], writes=[tBb])
            S.op("dve", lambda: nc.vector.tensor_reduce(out=ss[:, 0:4], in_=tB[:].rearrange("p (h e) -> p h e", h=4), axis=AX.X, op=ALU.add),
                 reads=[tBb], writes=[ssb])
            S.op("act", lambda: nc.scalar.activation(out=ss[:, 4:8], in_=ss[:, 0:4], func=AF.Sqrt, bias=C.epsc[:, 0:1], scale=1.0 / 128.0),
                 reads=[ssb, C.cstb], writes=[ssb])
            S.op("dve", lambda: nc.vector.reciprocal(out=ss[:, 4:8], in_=ss[:, 4:8]), reads=[ssb], writes=[ssb])
            S.op("dve", lambda: nc.vector.tensor_tensor(out=tA[:].rearrange("p (h e) -> p h e", h=4), in0=tA[:].rearrange("p (h e) -> p h e", h=4),
                                                        in1=bc(ss[:, 4:8].unsqueeze(2), [128, 4, 128]), op=ALU.mult), reads=[tAb, ssb], writes=[tAb])
            S.op("dve", lambda: nc.vector.tensor_tensor(out=tA[:], in0=tA[:], in1=nw[:], op=ALU.mult), reads=[tAb, nwb], writes=[tAb])
            S.op("dve", lambda: nc.vector.tensor_tensor(out=tA[:], in0=tA[:], in1=tZ[:], op=ALU.mult), reads=[tAb, tZb], writes=[tAb])
            for h in range(4):
                S.op("pe", lambda h=h: nc.tensor.transpose(ps[6][:, h * 128:(h + 1) * 128], tA[:, h * 128:(h + 1) * 128], C.cst[:, 0, :]),
                     reads=[tAb, C.cstb], writes=[psb[6]])
            S.op("dve", lambda t=t: nc.vector.tensor_copy(out=goT[:, :, t * 128:(t + 1) * 128], in_=ps[6][:, :].rearrange("p (h e) -> p h e", h=4)),
                 reads=[psb[6]], writes=[goTb])
        S.op("sp", lambda r0=r0, N=N: nc.sync.dma_start(out=oaT[:, :, 0:N], in_=Sx.oaT[:, :, r0:r0 + N].rearrange("c p t -> p c t")),
             reads=[C.b_oaT], writes=[oaTb], dma=True)
        for m in range(8):
            si = m % 2
            S.op("sp", lambda si=si, m=m, r0=r0, N=N: nc.sync.dma_start(out=sg[si][:, 0, 0:N], in_=Sx.sgT[m, :, r0:r0 + N]),
                 reads=[C.b_sgT], writes=[sgb[si]], dma=True)
            S.op("sp", lambda si=si, m=m, r0=r0, N=N: nc.sync.dma_start(out=sg[si][:, 1, 0:N], in_=Sx.sgT[8 + m, :, r0:r0 + N]),
                 reads=[C.b_sgT], writes=[sgb[si]], dma=True)
            pa, pb_ = (m % 2) * 2, (m % 2) * 2 + 1
            for c in range(4):
                S.op("pe", lambda c=c, m=m, pa=pa, N=N: nc.tensor.matmul(ps[pa][:, 0:N], wpa[:, c, m * 128:(m + 1) * 128], oaT[:, c, 0:N],
                                                                         start=(c == 0), stop=(c == 3)), reads=[wpab, oaTb], writes=[psb[pa]])
            for c in range(4):
                S.op("pe", lambda c=c, m=m, pb_=pb_, N=N: nc.tensor.matmul(ps[pb_][:, 0:N], wpb[:, c, m * 128:(m + 1) * 128], goT[:, c, 0:N],
                                                                           start=(c == 0), stop=(c == 3)), reads=[wpbb, goTb], writes=[psb[pb_]])
            S.op("dve", lambda si=si, pa=pa, N=N: nc.vector.tensor_tensor(out=tB[:, 0:N], in0=ps[pa][:, 0:N], in1=sg[si][:, 0, 0:N], op=ALU.mult),
                 reads=[psb[pa], sgb[si]], writes=[tBb])
            S.op("dve", lambda si=si, pb_=pb_, N=N: nc.vector.tensor_tensor(out=tZ[:, 0:N], in0=ps[pb_][:, 0:N], in1=sg[si][:, 1, 0:N], op=ALU.mult),
                 reads=[psb[pb_], sgb[si]], writes=[tZb])
            S.op("pool", lambda m=m, N=N: nc.gpsimd.tensor_tensor(out=yT[:, m, 0:N], in0=tB[:, 0:N], in1=tZ[:, 0:N], op=ALU.add),
                 reads=[tBb, tZb], writes=[yTb])
        for t in range(nt):
            rows = slice(r0 + t * 128, r0 + (t + 1) * 128)
            S.op("sp", lambda rows=rows: nc.sync.dma_start(out=xt[:], in_=xsrc[rows, :]), reads=[xsrcb], writes=[xtb], dma=True)
            for hh in range(2):
                for k in range(8):
                    S.op("pe", lambda k=k, hh=hh, t=t: nc.tensor.matmul(ps[4 + hh][:, :], yT[:, k, t * 128:(t + 1) * 128],
                                                                        wo[:, k, hh * 512:(hh + 1) * 512], start=(k == 0), stop=(k == 7)),
                         reads=[yTb, wob], writes=[psb[4 + hh]])
                S.op("dve", lambda hh=hh: nc.vector.tensor_tensor(out=rr[:, hh * 512:(hh + 1) * 512], in0=ps[4 + hh][:, :],
                                                                  in1=C.mod_bc[:, 0, isctx, hh * 512:(hh + 1) * 512], op=ALU.mult),
                     reads=[psb[4 + hh], C.mod_bcb], writes=[rrb])
            S.op("dve", lambda: nc.vector.scalar_tensor_tensor(out=rr[:], in0=xt[:], scalar=ALPHA, in1=rr[:], op0=ALU.mult, op1=ALU.add),
                 reads=[xtb, rrb], writes=[rrb])
            ln_affine(C, rr, rrb, x1t[:, t, :], x1tb, lnp[:, 0, :], lnp[:, 1, :], lnpb, st6, stb)
        ln_modT(C, l, x1t, x1tb, nt, 3, isctx, h2T, h2Tb, "e", xn, xnb, st6, stb, hT32=((h32, h32b) if moe else None),
                use_act=False)
        if moe:
            for t in range(nt):
                for k in range(8):
                    S.op("pe", lambda k=k, t=t: nc.tensor.matmul(ps[6][:, 0:NE], h32[:, k, t * 128:(t + 1) * 128], rt[:, k, :],
                                                                 start=(k == 0), stop=(k == 7)), reads=[h32b, rtb], writes=[psb[6]])
                S.op("dve", lambda t=t: nc.vector.tensor_copy(lg8[:, t, :], ps[6][:, 0:NE]), reads=[psb[6]], writes=[lg8b])
                S.op("dve", lambda t=t: nc.vector.tensor_reduce(out=mm[:, t, 0:1], in_=lg8[:, t, :], axis=AX.X, op=ALU.max), reads=[lg8b], writes=[mmb])
                S.op("dve", lambda t=t: nc.vector.tensor_scalar(out=eq[:, 0, t, :], in0=lg8[:, t, :], scalar1=mm[:, t, 0:1], scalar2=None,
                                                                op0=ALU.is_equal), reads=[lg8b, mmb], writes=[eqb])
                S.op("dve", lambda t=t: nc.vector.scalar_tensor_tensor(out=lgx[:, t, :], in0=eq[:, 0, t, :], scalar=-1.0e30, in1=lg8[:, t, :],
                                                                       op0=ALU.mult, op1=ALU.add), reads=[eqb, lg8b], writes=[lgxb])
                S.op("dve", lambda t=t: nc.vector.tensor_reduce(out=mm[:, t, 1:2], in_=lgx[:, t, :], axis=AX.X, op=ALU.max), reads=[lgxb], writes=[mmb])
                S.op("dve", lambda t=t: nc.vector.tensor_scalar(out=eq[:, 1, t, :], in0=lgx[:, t, :], scalar1=mm[:, t, 1:2], scalar2=None,
                                                                op0=ALU.is_equal), reads=[lgxb, mmb], writes=[eqb])
                S.op("dve", lambda t=t: nc.vector.tensor_tensor(out=mm[:, t, 2:3], in0=mm[:, t, 0:1], in1=mm[:, t, 1:2], op=ALU.subtract),
                     reads=[mmb], writes=[mmb])
                S.op("act", lambda t=t: nc.scalar.activation(out=mm[:, t, 2:3], in_=mm[:, t, 2:3], func=AF.Tanh, scale=0.5), reads=[mmb], writes=[mmb])
                S.op("dve", lambda t=t: nc.vector.tensor_scalar(out=mm[:, t, 2:3], in0=mm[:, t, 2:3], scalar1=0.5, scalar2=0.5,
                                                                op0=ALU.mult, op1=ALU.add), reads=[mmb], writes=[mmb])
                S.op("dve", lambda t=t: nc.vector.tensor_scalar(out=mm[:, t, 3:4], in0=mm[:, t, 2:3], scalar1=-1.0, scalar2=1.0,
                                                                op0=ALU.mult, op1=ALU.add), reads=[mmb], writes=[mmb])
                S.op("dve", lambda t=t: nc.vector.tensor_scalar_mul(out=gate[:, t, :], in0=eq[:, 0, t, :], scalar1=mm[:, t, 2:3]),
                     reads=[eqb, mmb], writes=[gateb])
                S.op("dve", lambda t=t: nc.vector.scalar_tensor_tensor(out=gate[:, t, :], in0=eq[:, 1, t, :], scalar=mm[:, t, 3:4], in1=gate[:, t, :],
                                                                       op0=ALU.mult, op1=ALU.add), reads=[eqb, mmb, gateb], writes=[gateb])
        experts = list(range(1, C.ne + 1)) if moe else [0]
        first = True
        for e in experts:
            for jb in range(DFF // 256):
                wi = wit % NWB
                wit += 1
                c0 = jb * 256
                S.op("sp", lambda wi=wi, e=e, c0=c0: nc.sync.dma_start(
                    out=w1b[wi][:], in_=Sx.wb1[e][:, c0:c0 + 256].rearrange("(k p) c -> p k c", p=128)),
                    reads=[C.wb[e]], writes=[wbb[wi]], dma=True)
                S.op("sp", lambda wi=wi, e=e, c0=c0: nc.sync.dma_start(
                    out=w3b[wi][:], in_=Sx.wb3[e][:, c0:c0 + 256].rearrange("(k p) c -> p k c", p=128)),
                    reads=[C.wb[e]], writes=[wbb[wi]], dma=True)
                S.op("sp", lambda wi=wi, e=e, c0=c0: nc.sync.dma_start(
                    out=w2b[wi][:], in_=Sx.wb2[e][c0:c0 + 256, :].rearrange("(c p) d -> p c d", p=128)),
                    reads=[C.wb[e]], writes=[wbb[wi]], dma=True)
                gi2 = jb % 2
                for c in range(2):
                    for k in range(8):
                        S.op("pe", lambda k=k, c=c, wi=wi, N=N: nc.tensor.matmul(ps[0 + 2 * c][:, 0:N], w1b[wi][:, k, c * 128:(c + 1) * 128],
                                                                                 h2T[:, k, 0:N], start=(k == 0), stop=(k == 7)),
                             reads=[wbb[wi], h2Tb], writes=[psb[0 + 2 * c]])
                    for k in range(8):
                        S.op("pe", lambda k=k, c=c, wi=wi, N=N: nc.tensor.matmul(ps[1 + 2 * c][:, 0:N], w3b[wi][:, k, c * 128:(c + 1) * 128],
                                                                                 h2T[:, k, 0:N], start=(k == 0), stop=(k == 7)),
                             reads=[wbb[wi], h2Tb], writes=[psb[1 + 2 * c]])
                    S.op("act", lambda c=c, N=N: nc.scalar.activation(out=sa[c][:, 0:N], in_=ps[0 + 2 * c][:, 0:N], func=AF.Silu),
                         reads=[psb[0 + 2 * c]], writes=[sab[c]])
                    S.op("dve", lambda c=c, gi2=gi2, N=N: nc.vector.tensor_tensor(out=gT[gi2][:, c, 0:N], in0=ps[1 + 2 * c][:, 0:N],
                                                                                  in1=sa[c][:, 0:N], op=ALU.mult),
                         reads=[psb[1 + 2 * c], sab[c]], writes=[gTb[gi2]])
                for t in range(nt):
                    for hh in range(2):
                        pbk = 4 + hh
                        for c in range(2):
                            S.op("pe", lambda c=c, t=t, hh=hh, wi=wi, gi2=gi2, pbk=pbk: nc.tensor.matmul(
                                ps[pbk][:, :], gT[gi2][:, c, t * 128:(t + 1) * 128], w2b[wi][:, c, hh * 512:(hh + 1) * 512],
                                start=(c == 0), stop=(c == 1)), reads=[gTb[gi2], wbb[wi]], writes=[psb[pbk]])
                        fa = facc[:, t, hh * 512:(hh + 1) * 512]
                        if moe:
                            gs = gate[:, t, e - 1:e]
                            if first:
                                S.op("dve", lambda fa=fa, gs=gs, pbk=pbk: nc.vector.tensor_scalar_mul(out=fa, in0=ps[pbk][:, :], scalar1=gs),
                                     reads=[psb[pbk], gateb], writes=[faccb])
                            else:
                                S.op("dve", lambda fa=fa, gs=gs, pbk=pbk: nc.vector.scalar_tensor_tensor(
                                    out=fa, in0=ps[pbk][:, :], scalar=gs, in1=fa, op0=ALU.mult, op1=ALU.add),
                                    reads=[psb[pbk], gateb, faccb], writes=[faccb])
                        else:
                            if first:
                                S.op("dve", lambda fa=fa, pbk=pbk: nc.vector.tensor_copy(out=fa, in_=ps[pbk][:, :]), reads=[psb[pbk]], writes=[faccb])
                            else:
                                S.op("dve", lambda fa=fa, pbk=pbk: nc.vector.tensor_tensor(out=fa, in0=ps[pbk][:, :], in1=fa, op=ALU.add),
                                     reads=[psb[pbk], faccb], writes=[faccb])
                first = False
        for t in range(nt):
            S.op("dve", lambda t=t: nc.vector.tensor_tensor(out=rr[:], in0=facc[:, t, :], in1=C.mod_bc[:, 1, isctx, :], op=ALU.mult),
                 reads=[faccb, C.mod_bcb], writes=[rrb])
            S.op("dve", lambda t=t: nc.vector.scalar_tensor_tensor(out=rr[:], in0=x1t[:, t, :], scalar=ALPHA, in1=rr[:], op0=ALU.mult, op1=ALU.add),
                 reads=[x1tb, rrb], writes=[rrb])
            ln_affine(C, rr, rrb, xt[:], xtb, lnp[:, 2, :], lnp[:, 3, :], lnpb, st6, stb)
            if l == 0:
                S.op("sp", lambda t=t, r0=r0: nc.sync.dma_start(out=Sx.x1[r0 + t * 128:r0 + (t + 1) * 128, :], in_=xt[:]),
                     reads=[xtb], writes=[dstb], dma=True)
            else:
                S.op("sp", lambda t=t, r0=r0: nc.sync.dma_start(out=C.y_out[r0 - TC + t * 128:r0 - TC + (t + 1) * 128, :], in_=xt[:]),
                     reads=[xtb], writes=[dstb], dma=True)


def ln_affine(C, src, srcb, dst, dstb, g, b, gbb, st6, stb):
    nc, S = C.nc, C.S
    for hh in range(2):
        S.op("dve", lambda hh=hh: nc.vector.bn_stats(out=st6[:, hh, :], in_=src[:, hh * 512:(hh + 1) * 512]), reads=[srcb], writes=[stb])
    S.op("dve", lambda: nc.vector.bn_aggr(out=st6[:, 2, 0:2], in_=st6[:, 0:2, :]), reads=[stb], writes=[stb])
    rstd_op(C, st6[:, 2, 2:3], st6[:, 2, 1:2], stb)
    S.op("dve", lambda: nc.vector.tensor_scalar(out=src[:], in0=src[:], scalar1=st6[:, 2, 0:1], scalar2=st6[:, 2, 2:3],
                                                op0=ALU.subtract, op1=ALU.mult), reads=[srcb, stb], writes=[srcb])
    S.op("dve", lambda: nc.vector.tensor_tensor(out=src[:], in0=src[:], in1=g, op=ALU.mult), reads=[srcb, gbb], writes=[srcb])
    S.op("dve", lambda: nc.vector.tensor_tensor(out=dst, in0=src[:], in1=b, op=ALU.add), reads=[srcb, gbb], writes=[dstb])


def make_consts():
    c = np.zeros((128, 8, 128), np.float32)
    r = np.arange(128)[:, None]
    q = np.arange(128)[None, :]
    c[:, 0, :] = (r == q)
    c[:, 1, :] = 1.0
    c[:, 2, :] = (r <= q)
    c[:, 3, :] = (r >= q)
    c[:, 4, :] = (r > q)
    c[:, 5, :] = (r < q)
    return c


def rowbc(v):
    v = np.asarray(v, np.float32).reshape(1, -1)
    return np.ascontiguousarray(np.broadcast_to(v, (128, v.shape[1])))


def prep_core_inputs(inp, b, half, TL, ne=NE, keys=None):
    f32 = lambda a: np.ascontiguousarray(np.asarray(a, dtype=np.float32))
    rev = (half == 1)
    dirs = [1, 0] if rev else [0, 1]

    def k_x():
        x_lat = np.asarray(inp["x"][b][:TL])
        x_ctx = np.asarray(inp["ctx"][b])
        if rev:
            x_lat = x_lat[::-1]
            x_ctx = x_ctx[::-1]
        return f32(np.concatenate([x_ctx, x_lat], axis=0))

    def k_sc():
        sc = np.stack([np.asarray(inp["c"][b]), np.asarray(inp["c_ctx"])], axis=-1)
        return f32(sc.reshape(8, 128, 2).transpose(1, 0, 2))

    def k_w_in():
        w_in = np.asarray(inp["w_in"])
        if rev:
            w_in = w_in.copy()
            tmp = w_in[:, :, 3072:3080].copy()
            w_in[:, :, 3072:3080] = w_in[:, :, 3080:3088]
            w_in[:, :, 3080:3088] = tmp
        return f32(w_in)

    def k_sgu_wT():
        sw = np.asarray(inp["sgu_w"])
        if rev:
            sw = sw[:, :, ::-1, ::-1]
        return f32(sw.transpose(0, 3, 1, 2))

    def k_sgu_b():
        sbias = np.asarray(inp["sgu_b"])
        if rev:
            sbias = sbias[:, :, ::-1]
        return f32(np.stack([rowbc(sbias[l].reshape(-1)).reshape(128, 4, 128) for l in range(2)]))

    def k_conv_w():
        cw = np.asarray(inp["conv_w"])
        if rev:
            cw = cw[:, ::-1, :]
        return f32(cw.reshape(2, 5, 12, 128).transpose(0, 3, 2, 1))

    def k_gate_par():
        gp = []
        for l in range(2):
            al = np.concatenate([np.asarray(inp["a_log"][l][d]) for d in dirs])
            db = np.concatenate([np.asarray(inp["dt_bias"][l][d]) for d in dirs])
            gp.append(np.stack([rowbc(al), rowbc(db)], axis=1))
        return f32(np.stack(gp))

    b_ada = lambda: np.asarray(inp["b_ada"])
    th = {
        "x": k_x, "sc": k_sc, "w_ada": lambda: f32(inp["w_ada"]),
        "b_ada_fm": lambda: f32(b_ada().reshape(2, 48, 128).transpose(0, 2, 1)),
        "b_ada_bc": lambda: f32(np.stack([rowbc(b_ada()[l]) for l in range(2)])),
        "w_in": k_w_in,
        "sgu_ln": lambda: f32(np.stack([np.stack([rowbc(inp["sgu_ln_g"][l]), rowbc(inp["sgu_ln_b"][l])], axis=1) for l in range(2)])),
        "sgu_wT": k_sgu_wT, "sgu_b": k_sgu_b, "conv_w": k_conv_w, "gate_par": k_gate_par,
        "gdn_nw": lambda: f32(np.stack([rowbc(np.tile(np.asarray(inp["gdn_norm_w"][l]), 4)) for l in range(2)])),
        "w_pa": lambda: f32(inp["w_pa"]), "w_pb": lambda: f32(inp["w_pb"]), "w_o": lambda: f32(inp["w_o"]),
        "lnp": lambda: f32(np.stack([np.stack([rowbc(inp[k][l]) for k in ("ln1_g", "ln1_b", "ln2_g", "ln2_b")], axis=1) for l in range(2)])),
        "ffn_w1": lambda: f32(inp["ffn_w1"][0]), "ffn_w3": lambda: f32(inp["ffn_w3"][0]), "ffn_w2": lambda: f32(inp["ffn_w2"][0]),
        "router": lambda: f32(np.asarray(inp["moe_router"][0]).reshape(8, 128, NE).transpose(1, 0, 2)),
        "moe_w1": lambda: f32(inp["moe_w1"][0][:ne]), "moe_w3": lambda: f32(inp["moe_w3"][0][:ne]),
        "moe_w2": lambda: f32(inp["moe_w2"][0][:ne]),
        "consts": make_consts,
    }
    if keys is None:
        keys = list(th)
    return {k: th[k]() for k in keys}


_CACHE = {}
FUSED = True


def get_programs(TL, ne=NE):
    key = (TL, ne, FUSED)
    if key not in _CACHE:
        if FUSED:
            _CACHE[key] = [build(TL, ne=ne)]
        else:
            _CACHE[key] = [build(TL, ne=ne, plan=p) for p in PLANS]
    return _CACHE[key]


def kernel(**inputs):
    x = np.asarray(inputs["x"])
    B, TL, _ = x.shape
    progs = get_programs(TL)
    ncores = 2 * B
    hand = [dict() for _ in range(ncores)]
    shared_cache = {}
    res = None
    for (nc, C) in progs:
        in_maps = []
        for b in range(B):
            for half in range(2):
                core = 2 * b + half
                keys = list(C.used_in)
                m = {}
                for k in keys:
                    if k in ("w_ada", "b_ada_fm", "b_ada_bc", "sgu_ln", "gdn_nw", "w_pa", "w_pb", "w_o", "lnp",
                             "ffn_w1", "ffn_w3", "ffn_w2", "router", "moe_w1", "moe_w3", "moe_w2", "consts"):
                        if k not in shared_cache:
                            shared_cache[k] = prep_core_inputs(inputs, b, half, TL, keys=[k])[k]
                        m[k] = shared_cache[k]
                    else:
                        m[k] = prep_core_inputs(inputs, b, half, TL, keys=[k])[k]
                for key in C.ext_in:
                    m["s_" + key] = hand[core]["s_" + key]
                in_maps.append(m)
        res = run_bass_kernel_spmd(nc, in_maps, core_ids=list(range(ncores)))
        for core in range(ncores):
            hand[core] = {("s_" + key): np.asarray(res.results[core]["s_" + key]) for key in C.ext_out}
        shared_cache = {k: v for k, v in shared_cache.items() if k == "consts"}
    out = np.zeros((B, TL, D), np.float32)
    OWN = TL // 2
    for b in range(B):
        y0 = np.asarray(res.results[2 * b]["y"])
        y1 = np.asarray(res.results[2 * b + 1]["y"])
        out[b, :OWN] = y0
        out[b, OWN:] = y1[::-1]
    return out
```

```python
import numpy as np
import concourse.bass as bass
import concourse.mybir as mybir
from concourse.bass_utils import run_bass_kernel_spmd
from contextlib import ExitStack

F32 = mybir.dt.float32
BF16 = mybir.dt.bfloat16
ALU = mybir.AluOpType
AF = mybir.ActivationFunctionType
AX = mybir.AxisListType

D = 1024
TC = 256
NIN = 5136
DFF = 3584
NE = 8
LN_EPS = 1e-6
DEPTH = 2
ALPHA = (2.0 * DEPTH) ** 0.25


class Buf:
    __slots__ = ("name", "w", "r")

    def __init__(self, name):
        self.name = name
        self.w = None
        self.r = {}


class Sch:
    ENG = ("pe", "dve", "act", "pool", "sp")
    NDMA = 40
    NSW = 8

    def __init__(self, nc, stack, needed=None):
        self.nc = nc
        self.e = {"pe": nc.tensor, "dve": nc.vector, "act": nc.scalar, "pool": nc.gpsimd, "sp": nc.sync}
        self.sem = {}
        self.cnt = {}
        for k in self.ENG:
            self.sem[k] = stack.enter_context(nc.semaphore("sem_" + k))
            self.cnt[k] = 0
        for j in range(self.NDMA):
            k = "d%d" % j
            self.sem[k] = stack.enter_context(nc.semaphore("sem_" + k))
            self.cnt[k] = 0
        self.rr = 0
        self.rr_sw = 0
        self.waited = {k: {} for k in self.ENG}
        self.nops = 0
        self.nwaits = 0
        self.ninst = {k: 0 for k in self.ENG}
        self.record = needed is None
        self.needed = {k: set() for k in self.ENG} if needed is None else needed
        if not self.record:
            self.rank = {}
            for k in self.ENG:
                srt = sorted(self.needed[k])
                self.rank[k] = {v: i + 1 for i, v in enumerate(srt)}

    def _wait(self, eng, key, val):
        if val <= 0:
            return
        if self.waited[eng].get(key, 0) >= val:
            return
        self.waited[eng][key] = val
        self.nwaits += 1
        self.ninst[eng] += 1
        if key in self.needed:
            if self.record:
                self.needed[key].add(val)
                return
            sv = self.rank[key][val]
        else:
            sv = val
        self.e[eng].wait_ge(self.sem[key], sv)

    def op(self, eng, fn, reads=(), writes=(), dma=False):
        deps = {}
        for b in reads:
            if b.w is not None:
                k, v = b.w
                if deps.get(k, 0) < v:
                    deps[k] = v
        for b in writes:
            if b.w is not None:
                k, v = b.w
                if deps.get(k, 0) < v:
                    deps[k] = v
            for k, v in b.r.items():
                if deps.get(k, 0) < v:
                    deps[k] = v
        for k, v in deps.items():
            if eng == "pe" and k == "pe" and not dma:
                continue
            self._wait(eng, k, v)
        if dma:
            if eng == "pool":
                j = self.NDMA - self.NSW + self.rr_sw
                self.rr_sw = (self.rr_sw + 1) % self.NSW
            else:
                j = self.rr
                self.rr = (self.rr + 1) % (self.NDMA - self.NSW)
            key = "d%d" % j
            self._wait(eng, key, self.cnt[key])
            inst = fn()
            inst.then_inc(self.sem[key], 16)
            self.cnt[key] += 16
            tok = (key, self.cnt[key])
        else:
            inst = fn()
            self.cnt[eng] += 1
            if (not self.record) and (self.cnt[eng] in self.rank[eng]):
                inst.then_inc(self.sem[eng], 1)
            tok = (eng, self.cnt[eng])
        for b in writes:
            b.w = tok
            b.r = {}
        for b in reads:
            k, v = tok
            if b.r.get(k, 0) < v:
                b.r[k] = v
        self.nops += 1
        self.ninst[eng] += 1
        return inst

    def barrier(self):
        for eng in self.ENG:
            for k, v in self.cnt.items():
                if k == eng:
                    continue
                self._wait(eng, k, v)

    def final_wait(self, eng="sp"):
        for k, v in self.cnt.items():
            if k != eng:
                self._wait(eng, k, v)


class Ctx:
    def __getattr__(self, name):
        if name.startswith("b_"):
            b = Buf(name)
            object.__setattr__(self, name, b)
            return b
        raise AttributeError(name)


def bc(ap, shape):
    return ap.broadcast_to(list(shape))


def rstd_op(C, out, in_, buf, eps=LN_EPS, extra_reads=()):
    nc, S = C.nc, C.S
    S.op("act", lambda: nc.scalar.activation(out=out, in_=in_, func=AF.Sqrt, bias=C.epsc[:in_.shape[0], 0:1], scale=1.0),
         reads=[buf, C.cstb] + list(extra_reads), writes=[buf])
    S.op("dve", lambda: nc.vector.reciprocal(out=out, in_=out), reads=[buf], writes=[buf])


ALL_STEPS = [(p, l) for l in range(DEPTH) for p in ("mod", "a", "b", "cd", "e")]
PLANS = [
    dict(steps=[("mod", 0), ("a", 0), ("b", 0), ("cd", 0)], ext_in=[], ext_out=["oaT0", "sgT0", "zs0", "of0", "ob0"], wconv=[]),
    dict(steps=[("mod", 0), ("e", 0), ("mod", 1), ("a", 1), ("b", 1), ("cd", 1)],
         ext_in=["oaT0", "sgT0", "zs0", "of0", "ob0"], ext_out=["x1", "oaT1", "sgT1", "zs1", "of1", "ob1"], wconv=[0]),
    dict(steps=[("mod", 1), ("e", 1)], ext_in=["x1", "oaT1", "sgT1", "zs1", "of1", "ob1"], ext_out=[],
         wconv=list(range(1, NE + 1))),
]


def build(TL, debug=False, stop_after=None, ne=NE, plan=None):
    if plan is None:
        steps = []
        for st in ALL_STEPS:
            steps.append(st)
            if stop_after is not None and st == stop_after:
                break
        plan = dict(steps=steps, ext_in=[], ext_out=[], wconv=list(range(ne + 1)))
    _, C1 = build1(TL, debug, ne, plan, None)
    return build1(TL, debug, ne, plan, C1.S.needed)


class Lazy:
    def __init__(self, make):
        object.__setattr__(self, "_make", make)

    def __getattr__(self, name):
        v = self._make(name)
        object.__setattr__(self, name, v)
        return v


def build1(TL, debug, ne, plan, needed):
    T = TC + TL
    OWN = TL // 2
    NCH = T // 64
    nc = bass.Bass("TRN2", target_bir_lowering=False)
    C = Ctx()
    C.nc = nc
    C.TL, C.T, C.OWN, C.NCH = TL, T, OWN, NCH
    C.debug = debug
    C.ne = ne
    C.used_in = []
    C.ext_in = list(plan["ext_in"])
    C.ext_out = list(plan["ext_out"])
    in_specs = {
        "x": ([T, D], F32), "sc": ([128, 8, 2], F32), "w_ada": ([2, D, 6 * D], F32), "b_ada_fm": ([2, 128, 48], F32),
        "b_ada_bc": ([2, 128, 6 * D], F32), "w_in": ([2, D, NIN], F32), "sgu_ln": ([2, 128, 2, 512], F32),
        "sgu_wT": ([2, 128, 4, 128], F32), "sgu_b": ([2, 128, 4, 128], F32), "conv_w": ([2, 128, 12, 5], F32),
        "gate_par": ([2, 128, 2, 8], F32), "gdn_nw": ([2, 128, 512], F32), "w_pa": ([2, 512, D], F32),
        "w_pb": ([2, 512, D], F32), "w_o": ([2, D, D], F32), "lnp": ([2, 128, 4, D], F32),
        "ffn_w1": ([D, DFF], F32), "ffn_w3": ([D, DFF], F32), "ffn_w2": ([DFF, D], F32), "router": ([128, 8, NE], F32),
        "moe_w1": ([ne, D, DFF], F32), "moe_w3": ([ne, D, DFF], F32), "moe_w2": ([ne, DFF, D], F32),
        "consts": ([128, 8, 128], F32),
    }

    def make_in(name):
        shape, dt = in_specs[name]
        C.used_in.append(name)
        return nc.dram_tensor(name, list(shape), dt, kind="ExternalInput").ap()

    C.I = Lazy(make_in)
    sc_specs = {
        "x1": ([T, D], F32), "qkvT": ([12, 128, T], F32), "qkn": ([8, 128, T], F32), "ktok": ([T, 512], F32),
        "vtok": ([T, 512], F32), "glb": ([T, 16], F32), "oaT": ([4, 128, T], BF16), "sgT": ([16, 128, T], BF16),
        "zs": ([T, 512], F32), "of": ([T, 512], F32), "ob": ([T, 512], F32),
        "wb1": ([ne + 1, D, DFF], BF16), "wb3": ([ne + 1, D, DFF], BF16), "wb2": ([ne + 1, DFF, D], BF16),
    }
    shared = {}
    C.ext_names = {}

    def make_sc(l):
        def mk(name):
            key = name if name in ("x1", "wb1", "wb3", "wb2") else name + str(l)
            if key in shared:
                return shared[key]
            shape, dt = sc_specs[name]
            if key in C.ext_in:
                kind = "ExternalInput"
            elif key in C.ext_out or (debug and not name.startswith("wb")):
                kind = "ExternalOutput"
            else:
                kind = "Internal"
            t = nc.dram_tensor("s_" + key, list(shape), dt, kind=kind).ap()
            shared[key] = t
            C.ext_names[key] = "s_" + key
            return t
        return mk

    C.SxL = [Lazy(make_sc(l)) for l in range(DEPTH)]
    C.Sx = C.SxL[0]
    steps = plan["steps"]
    if ("e", DEPTH - 1) in steps:
        C.y_out = nc.dram_tensor("y", [OWN, D], F32, kind="ExternalOutput").ap()

    with ExitStack() as stack:
        S = Sch(nc, stack, needed)
        C.S = S
        C.stack = stack
        C.ps = [stack.enter_context(nc.psum_tensor("ps%d" % i, [128, 512], F32)) for i in range(8)]
        C.psb = [Buf("ps%d" % i) for i in range(8)]
        C.cst = stack.enter_context(nc.sbuf_tensor("cst", [128, 8, 128], F32))
        C.cstb = Buf("cst")
        S.op("sp", lambda: nc.sync.dma_start(out=C.cst[:], in_=C.I.consts[:, :, :]), writes=[C.cstb], dma=True)
        C.epsc = stack.enter_context(nc.sbuf_tensor("epsc", [128, 1], F32))
        S.op("dve", lambda: nc.vector.memset(C.epsc[:], LN_EPS), writes=[C.cstb])

        C.wb = [Buf("wb%d" % e) for e in range(ne + 1)]
        weight_convert(C, [e for e in plan["wconv"] if e <= ne])
        fns = {"mod": modulation, "a": pass_a, "b": pass_b, "cd": pass_cd, "e": pass_e}
        for (p, l) in steps:
            C.Sx = C.SxL[l]
            with ExitStack() as ls:
                C.ls = ls
                fns[p](C, l)
                S.barrier()
        import os
        for _i in range(int(os.environ.get("EXTRA_ACT", "0"))):
            S.op("act", lambda: nc.scalar.copy(out=C.epsc[:, 0:1], in_=C.epsc[:, 0:1]), reads=[], writes=[])
        for _i in range(int(os.environ.get("EXTRA_DVE", "0"))):
            S.op("dve", lambda: nc.vector.tensor_copy(C.epsc[:, 0:1], C.epsc[:, 0:1]), reads=[], writes=[])
        S.final_wait("sp")
        S.final_wait("act")
    C.nc = nc
    return nc, C


def weight_convert(C, which):
    nc, S, I, Sx = C.nc, C.S, C.I, C.Sx
    for e in which:
        if e == 0:
            a, b, c = I.ffn_w1, I.ffn_w3, I.ffn_w2
        else:
            a, b, c = I.moe_w1[e - 1], I.moe_w3[e - 1], I.moe_w2[e - 1]
        for (dst, src, rows) in ((Sx.wb1[e], a, D), (Sx.wb3[e], b, D), (Sx.wb2[e], c, DFF)):
            step = 512
            for r0 in range(0, rows, step):
                S.op("pool", lambda dst=dst, src=src, r0=r0, step=step: nc.gpsimd.dma_start(
                    out=dst[r0:r0 + step, :], in_=src[r0:r0 + step, :]), writes=[C.wb[e]], dma=True)


def modulation(C, l):
    nc, S, I = C.nc, C.S, C.I
    ls = C.ls
    if not hasattr(C, "mod_fm"):
        st = C.stack
        C.mod_fm = st.enter_context(nc.sbuf_tensor("mod_fm", [128, 48, 2], F32))
        C.mod_fmb = Buf("mod_fm")
        C.mod_bc = st.enter_context(nc.sbuf_tensor("mod_bc", [128, 2, 2, D], F32))
        C.mod_bcb = Buf("mod_bc")
    C.sil = ls.enter_context(nc.sbuf_tensor("sil%d" % l, [128, 8, 2], F32))
    C.silb = Buf("sil")
    C.silB = ls.enter_context(nc.sbuf_tensor("silB%d" % l, [128, 8, 2, 128], F32))
    C.silBb = Buf("silB")
    C.bfm = ls.enter_context(nc.sbuf_tensor("bfm%d" % l, [128, 48], F32))
    C.bfmb = Buf("bfm")
    S.op("sp", lambda: nc.sync.dma_start(out=C.sil[:], in_=I.sc[:, :, :]), writes=[C.silb], dma=True)
    S.op("act", lambda: nc.scalar.activation(out=C.sil[:], in_=C.sil[:], func=AF.Silu), reads=[C.silb], writes=[C.silb])
    for w in range(2):
        S.op("dve", lambda w=w: nc.vector.tensor_copy(
            C.silB[:, :, w, :], bc(C.sil[:, :, w:w + 1], [128, 8, 128])), reads=[C.silb], writes=[C.silBb])
    C.wada = [ls.enter_context(nc.sbuf_tensor("wada%d_%d" % (i, l), [128, 8, 512], F32)) for i in range(2)]
    C.wadab = [Buf("wada%d" % i) for i in range(2)]
    S.op("sp", lambda: nc.sync.dma_start(out=C.bfm[:], in_=I.b_ada_fm[l]), writes=[C.bfmb], dma=True)
    for w, which in enumerate((2, 5)):
        S.op("sp", lambda w=w, which=which: nc.sync.dma_start(
            out=C.mod_bc[:, w, 0, :], in_=I.b_ada_bc[l][:, which * D:(which + 1) * D]), writes=[C.mod_bcb], dma=True)
        S.op("sp", lambda w=w, which=which: nc.sync.dma_start(
            out=C.mod_bc[:, w, 1, :], in_=I.b_ada_bc[l][:, which * D:(which + 1) * D]), writes=[C.mod_bcb], dma=True)
    pfm, pbc = C.ps[0], C.ps[1]
    for blk in range(12):
        wt, wtb = C.wada[blk % 2], C.wadab[blk % 2]
        S.op("sp", lambda wt=wt, blk=blk: nc.sync.dma_start(
            out=wt[:], in_=I.w_ada[l][:, blk * 512:(blk + 1) * 512].rearrange("(k p) c -> p k c", p=128)),
            writes=[wtb], dma=True)
        which = blk // 2
        if which in (2, 5):
            w = 0 if which == 2 else 1
            for lc in range(2):
                for k in range(8):
                    S.op("pe", lambda k=k, lc=lc, wt=wt: nc.tensor.matmul(
                        pbc[:, :], C.silB[:, k, lc, :], wt[:, k, :], start=(k == 0), stop=(k == 7)),
                        reads=[C.silBb, wtb], writes=[C.psb[1]])
                S.op("dve", lambda w=w, lc=lc, blk=blk: nc.vector.tensor_tensor(
                    out=C.mod_bc[:, w, lc, (blk % 2) * 512:(blk % 2 + 1) * 512],
                    in0=C.mod_bc[:, w, lc, (blk % 2) * 512:(blk % 2 + 1) * 512], in1=pbc[:, :], op=ALU.add),
                    reads=[C.psb[1], C.mod_bcb], writes=[C.mod_bcb])
        else:
            for cc in range(4):
                j = blk * 4 + cc
                for k in range(8):
                    S.op("pe", lambda k=k, cc=cc, wt=wt: nc.tensor.matmul(
                        pfm[:, cc * 2:cc * 2 + 2], wt[:, k, cc * 128:(cc + 1) * 128], C.sil[:, k, :],
                        start=(k == 0), stop=(k == 7)), reads=[C.silb, wtb], writes=[C.psb[0]])
            for cc in range(4):
                j = blk * 4 + cc
                S.op("dve", lambda j=j, cc=cc: nc.vector.tensor_tensor(
                    out=C.mod_fm[:, j, :], in0=pfm[:, cc * 2:cc * 2 + 2], in1=bc(C.bfm[:, j:j + 1], [128, 2]),
                    op=ALU.add), reads=[C.psb[0], C.bfmb], writes=[C.mod_fmb])
    for j0 in (8, 32):
        S.op("dve", lambda j0=j0: nc.vector.tensor_scalar_add(
            out=C.mod_fm[:, j0:j0 + 8, :], in0=C.mod_fm[:, j0:j0 + 8, :], scalar1=1.0),
            reads=[C.mod_fmb], writes=[C.mod_fmb])


def groups_of(C, gsz):
    g = [(0, 2, 1)]
    r = TC
    while r < C.T:
        n = min(gsz, (C.T - r) // 128)
        g.append((r, n, 0))
        r += n * 128
    return g


def ln_modT(C, l, xs, xsb, nt, which, isctx, hT, hTb, tagsfx, xn, xnb, st6, stb, hT32=None, use_act=True):
    nc, S = C.nc, C.S
    jsh, jsc = which * 8, which * 8 + 8
    for t in range(nt):
        for hh in range(2):
            S.op("dve", lambda t=t, hh=hh: nc.vector.bn_stats(out=st6[:, hh, :], in_=xs[:, t, hh * 512:(hh + 1) * 512]),
                 reads=[xsb], writes=[stb])
        S.op("dve", lambda: nc.vector.bn_aggr(out=st6[:, 2, 0:2], in_=st6[:, 0:2, :]), reads=[stb], writes=[stb])
        rstd_op(C, st6[:, 2, 2:3], st6[:, 2, 1:2], stb)
        S.op("dve", lambda t=t: nc.vector.tensor_scalar(out=xn[:], in0=xs[:, t, :], scalar1=st6[:, 2, 0:1],
                                                        scalar2=st6[:, 2, 2:3], op0=ALU.subtract, op1=ALU.mult),
             reads=[xsb, stb], writes=[xnb])
        for half in range(2):
            pb = 6 + half
            for kk in range(4):
                k = half * 4 + kk
                S.op("pe", lambda k=k, kk=kk, pb=pb: nc.tensor.transpose(
                    C.ps[pb][:, kk * 128:(kk + 1) * 128], xn[:, k * 128:(k + 1) * 128], C.cst[:, 0, :]),
                    reads=[xnb, C.cstb], writes=[C.psb[pb]])
            for kk in range(4):
                k = half * 4 + kk
                eng = "dve" if (kk % 2 == 0 or not use_act) else "act"
                if eng == "dve":
                    S.op("dve", lambda k=k, kk=kk, pb=pb, t=t: nc.vector.tensor_scalar(
                        out=hT[:, k, t * 128:(t + 1) * 128], in0=C.ps[pb][:, kk * 128:(kk + 1) * 128],
                        scalar1=C.mod_fm[:, jsc + k, isctx:isctx + 1], scalar2=C.mod_fm[:, jsh + k, isctx:isctx + 1],
                        op0=ALU.mult, op1=ALU.add), reads=[C.psb[pb], C.mod_fmb], writes=[hTb])
                else:
                    S.op("act", lambda k=k, kk=kk, pb=pb, t=t: nc.scalar.activation(
                        out=hT[:, k, t * 128:(t + 1) * 128], in_=C.ps[pb][:, kk * 128:(kk + 1) * 128],
                        func=AF.Identity, scale=C.mod_fm[:, jsc + k, isctx:isctx + 1],
                        bias=C.mod_fm[:, jsh + k, isctx:isctx + 1]), reads=[C.psb[pb], C.mod_fmb], writes=[hTb])
                if hT32 is not None:
                    S.op("dve", lambda k=k, kk=kk, pb=pb, t=t: nc.vector.tensor_scalar(
                        out=hT32[0][:, k, t * 128:(t + 1) * 128], in0=C.ps[pb][:, kk * 128:(kk + 1) * 128],
                        scalar1=C.mod_fm[:, jsc + k, isctx:isctx + 1], scalar2=C.mod_fm[:, jsh + k, isctx:isctx + 1],
                        op0=ALU.mult, op1=ALU.add), reads=[C.psb[pb], C.mod_fmb], writes=[hT32[1]])


def pass_a(C, l):
    nc, S, I, Sx, ls = C.nc, C.S, C.I, C.Sx, C.ls
    T = C.T
    xsrc = I.x if l == 0 else Sx.x1
    sb = lambda name, shape, dt=F32: ls.enter_context(nc.sbuf_tensor(name + "_a%d" % l, list(shape), dt))
    win = sb("win", [128, 8, NIN], BF16)
    winb = Buf("win")
    for k in range(8):
        S.op("pool", lambda k=k: nc.gpsimd.dma_start(out=win[:, k, :], in_=I.w_in[l][k * 128:(k + 1) * 128, :]),
             writes=[winb], dma=True)
    sln = sb("sln", [128, 2, 512]); slnb = Buf("sln")
    S.op("sp", lambda: nc.sync.dma_start(out=sln[:], in_=I.sgu_ln[l]), writes=[slnb], dma=True)
    swT32 = sb("swT32", [128, 4, 128]); swT = sb("swT", [128, 4, 128], BF16); swTb = Buf("swT")
    S.op("sp", lambda: nc.sync.dma_start(out=swT32[:], in_=I.sgu_wT[l]), writes=[swTb], dma=True)
    S.op("dve", lambda: nc.vector.tensor_copy(swT[:], swT32[:]), reads=[swTb], writes=[swTb])
    sbias = sb("sbias", [128, 4, 128]); sbiasb = Buf("sbias")
    S.op("sp", lambda: nc.sync.dma_start(out=sbias[:], in_=I.sgu_b[l]), writes=[sbiasb], dma=True)
    gpar = sb("gpar", [128, 2, 8]); gparb = Buf("gpar")
    S.op("sp", lambda: nc.sync.dma_start(out=gpar[:], in_=I.gate_par[l]), writes=[gparb], dma=True)
    S.op("act", lambda: nc.scalar.activation(out=gpar[:, 0, :], in_=gpar[:, 0, :], func=AF.Exp), reads=[gparb], writes=[gparb])
    S.op("dve", lambda: nc.vector.tensor_scalar_mul(out=gpar[:, 0, :], in0=gpar[:, 0, :], scalar1=-1.0), reads=[gparb], writes=[gparb])

    NB = 2
    xs = [sb("xs%d" % i, [128, 4, D]) for i in range(NB)]; xsb = [Buf("xs%d" % i) for i in range(NB)]
    hT = [sb("hT%d" % i, [128, 8, 512], BF16) for i in range(NB)]; hTb = [Buf("hT%d" % i) for i in range(NB)]
    xn = sb("xn", [128, D]); xnb = Buf("xn")
    st6 = sb("st6", [128, 3, 6]); stb = Buf("st6")
    uT = sb("uT", [128, 4, 512], BF16); uTb = Buf("uT")
    oaT = [sb("oaT%d" % i, [128, 4, 512], BF16) for i in range(NB)]; oaTb = [Buf("oaT%d" % i) for i in range(NB)]
    stg = [sb("stg%d" % i, [128, 512]) for i in range(4)]; stgb = [Buf("stg%d" % i) for i in range(4)]
    sgs = [sb("sgs%d" % i, [128, 512], BF16) for i in range(4)]; sgsb = [Buf("sgs%d" % i) for i in range(4)]
    vg = sb("vg", [128, 512]); vgb = Buf("vg")
    vn = sb("vn", [128, 512], BF16); vnb = Buf("vn")
    zst = [sb("zst%d" % i, [128, 512]) for i in range(2)]; zstb = [Buf("zst%d" % i) for i in range(2)]
    gt = sb("gt", [128, 4, 16]); gtb = Buf("gt")
    gw = sb("gw", [128, 4, 8]); gwb = Buf("gw")
    mx = sb("mx", [128, 512]); mxb = Buf("mx")

    qkvb = Buf("s_qkvT"); C.b_qkvT = qkvb
    C.b_oaT = Buf("s_oaT"); C.b_sgT = Buf("s_sgT"); C.b_zs = Buf("s_zs"); C.b_glb = Buf("s_glb")
    C.b_x1 = getattr(C, "b_x1", Buf("s_x1"))
    xsrcb = C.b_x1 if l == 1 else Buf("xin")
    own_hi = TC + C.OWN

    col_u, col_v, col_qkv, col_z, col_g, col_ga, col_gb = 0, 512, 1024, 2560, 3072, 3088, 4112
    rot = 0
    stc = 0
    for gi, (r0, nt, isctx) in enumerate(groups_of(C, 4)):
        N = nt * 128
        full = (l == 0) or (not isctx and r0 < own_hi)
        b = gi % NB
        S.op("sp", lambda b=b, r0=r0, nt=nt: nc.sync.dma_start(
            out=xs[b][:, 0:nt, :], in_=xsrc[r0:r0 + nt * 128, :].rearrange("(t p) d -> p t d", p=128)),
            reads=[xsrcb], writes=[xsb[b]], dma=True)
        ln_modT(C, l, xs[b], xsb[b], nt, 0, isctx, hT[b], hTb[b], "a", xn, xnb, st6, stb, use_act=False)
        fm = []
        if full:
            fm += [("u", c, col_u + c * 128) for c in range(4)]
        fm += [("qkv", c, col_qkv + c * 128) for c in range(12)]
        if full:
            fm += [("ga", c, col_ga + c * 128) for c in range(8)] + [("gb", c, col_gb + c * 128) for c in range(8)]
        for (kind, c, col) in fm:
            pb = rot % 4
            rot += 1
            for k in range(8):
                S.op("pe", lambda k=k, col=col, pb=pb, b=b, N=N: nc.tensor.matmul(
                    C.ps[pb][:, 0:N], win[:, k, col:col + 128], hT[b][:, k, 0:N], start=(k == 0), stop=(k == 7)),
                    reads=[winb, hTb[b]], writes=[C.psb[pb]])
            if kind == "u":
                S.op("act", lambda c=c, pb=pb, N=N: nc.scalar.activation(out=uT[:, c, 0:N], in_=C.ps[pb][:, 0:N], func=AF.Gelu),
                     reads=[C.psb[pb]], writes=[uTb])
            elif kind == "qkv":
                si = stc % 4
                stc += 1
                if c % 2 == 0:
                    S.op("dve", lambda si=si, pb=pb, N=N: nc.vector.tensor_copy(stg[si][:, 0:N], C.ps[pb][:, 0:N]),
                         reads=[C.psb[pb]], writes=[stgb[si]])
                else:
                    S.op("dve", lambda si=si, pb=pb, N=N: nc.vector.tensor_copy(out=stg[si][:, 0:N], in_=C.ps[pb][:, 0:N]),
                         reads=[C.psb[pb]], writes=[stgb[si]])
                S.op("sp", lambda si=si, c=c, r0=r0, N=N: nc.sync.dma_start(out=Sx.qkvT[c, :, r0:r0 + N], in_=stg[si][:, 0:N]),
                     reads=[stgb[si]], writes=[qkvb], dma=True)
            else:
                si = stc % 4
                stc += 1
                cc = c + (8 if kind == "gb" else 0)
                S.op("act", lambda si=si, pb=pb, N=N: nc.scalar.activation(out=sgs[si][:, 0:N], in_=C.ps[pb][:, 0:N], func=AF.Sigmoid),
                     reads=[C.psb[pb]], writes=[sgsb[si]])
                S.op("sp", lambda si=si, cc=cc, r0=r0, N=N: nc.sync.dma_start(out=Sx.sgT[cc, :, r0:r0 + N], in_=sgs[si][:, 0:N]),
                     reads=[sgsb[si]], writes=[C.b_sgT], dma=True)
        for t in range(nt):
            tl = slice(t * 128, (t + 1) * 128)
            pb = 4
            for k in range(8):
                S.op("pe", lambda k=k, b=b, tl=tl: nc.tensor.matmul(
                    C.ps[4][:, 0:16], hT[b][:, k, tl], win[:, k, col_g:col_g + 16], start=(k == 0), stop=(k == 7)),
                    reads=[winb, hTb[b]], writes=[C.psb[4]])
            S.op("dve", lambda t=t: nc.vector.tensor_copy(gt[:, t, :], C.ps[4][:, 0:16]), reads=[C.psb[4]], writes=[gtb])
            if full:
                for k in range(8):
                    S.op("pe", lambda k=k, b=b, tl=tl: nc.tensor.matmul(
                        C.ps[5][:, :], hT[b][:, k, tl], win[:, k, col_v:col_v + 512], start=(k == 0), stop=(k == 7)),
                        reads=[winb, hTb[b]], writes=[C.psb[5]])
                S.op("act", lambda: nc.scalar.activation(out=vg[:], in_=C.ps[5][:, :], func=AF.Gelu), reads=[C.psb[5]], writes=[vgb])
                S.op("dve", lambda: nc.vector.bn_stats(out=st6[:, 0, :], in_=vg[:]), reads=[vgb], writes=[stb])
                S.op("dve", lambda: nc.vector.bn_aggr(out=st6[:, 2, 0:2], in_=st6[:, 0:1, :]), reads=[stb], writes=[stb])
                rstd_op(C, st6[:, 2, 2:3], st6[:, 2, 1:2], stb)
                S.op("dve", lambda: nc.vector.tensor_scalar(out=vg[:], in0=vg[:], scalar1=st6[:, 2, 0:1], scalar2=st6[:, 2, 2:3],
                                                            op0=ALU.subtract, op1=ALU.mult), reads=[vgb, stb], writes=[vgb])
                S.op("dve", lambda: nc.vector.tensor_tensor(out=vg[:], in0=vg[:], in1=sln[:, 0, :], op=ALU.mult),
                     reads=[vgb, slnb], writes=[vgb])
                S.op("dve", lambda: nc.vector.tensor_tensor(out=vn[:], in0=vg[:], in1=sln[:, 1, :], op=ALU.add),
                     reads=[vgb, slnb], writes=[vnb])
                for g in range(4):
                    S.op("pe", lambda g=g: nc.tensor.matmul(C.ps[5][:, g * 128:(g + 1) * 128], vn[:, g * 128:(g + 1) * 128],
                                                            swT[:, g, :], start=True, stop=True),
                         reads=[vnb, swTb], writes=[C.psb[5]])
                S.op("dve", lambda: nc.vector.tensor_tensor(out=mx[:], in0=C.ps[5][:, :], in1=sbias[:].rearrange("p g q -> p (g q)"),
                                                            op=ALU.add), reads=[C.psb[5], sbiasb], writes=[mxb])
                S.op("dve", lambda b=b, tl=tl: nc.vector.tensor_tensor(
                    out=oaT[b][:, :, tl], in0=mx[:].rearrange("p (g q) -> p g q", g=4), in1=uT[:, :, tl], op=ALU.mult),
                    reads=[mxb, uTb], writes=[oaTb[b]])
                zi = t % 2
                for k in range(8):
                    S.op("pe", lambda k=k, b=b, tl=tl: nc.tensor.matmul(
                        C.ps[5][:, :], hT[b][:, k, tl], win[:, k, col_z:col_z + 512], start=(k == 0), stop=(k == 7)),
                        reads=[winb, hTb[b]], writes=[C.psb[5]])
                S.op("act", lambda zi=zi: nc.scalar.activation(out=zst[zi][:], in_=C.ps[5][:, :], func=AF.Silu),
                     reads=[C.psb[5]], writes=[zstb[zi]])
                S.op("sp", lambda zi=zi, r0=r0, t=t: nc.sync.dma_start(out=Sx.zs[r0 + t * 128:r0 + (t + 1) * 128, :], in_=zst[zi][:]),
                     reads=[zstb[zi]], writes=[C.b_zs], dma=True)
        if full:
            S.op("sp", lambda b=b, r0=r0, N=N: nc.sync.dma_start(
                out=Sx.oaT[:, :, r0:r0 + N].rearrange("c p t -> p c t"), in_=oaT[b][:, :, 0:N]),
                reads=[oaTb[b]], writes=[C.b_oaT], dma=True)
        gv = gt[:, 0:nt, :].rearrange("p t (d k h) -> p t d k h", d=2, k=2)
        a_v = gv[:, :, :, 0, :]
        b_v = gv[:, :, :, 1, :]
        gwv = gw[:, 0:nt, :].rearrange("p t (d h) -> p t d h", d=2)
        dtb = bc(gpar[:, 1:2, :], [128, nt, 8]).rearrange("p t (d h) -> p t d h", d=2)
        nea = bc(gpar[:, 0:1, :], [128, nt, 8]).rearrange("p t (d h) -> p t d h", d=2)
        S.op("dve", lambda: nc.vector.tensor_tensor(out=a_v, in0=a_v, in1=dtb, op=ALU.add), reads=[gtb, gparb], writes=[gtb])
        S.op("act", lambda: nc.scalar.activation(out=gwv, in_=a_v, func=AF.Abs), reads=[gtb], writes=[gwb])
        S.op("act", lambda: nc.scalar.activation(out=gw[:, 0:nt, :], in_=gw[:, 0:nt, :], func=AF.Exp, scale=-1.0), reads=[gwb], writes=[gwb])
        S.op("act", lambda: nc.scalar.activation(out=gw[:, 0:nt, :], in_=gw[:, 0:nt, :], func=AF.Ln, bias=1.0), reads=[gwb], writes=[gwb])
        S.op("dve", lambda: nc.vector.tensor_scalar_max(out=a_v, in0=a_v, scalar1=0.0), reads=[gtb], writes=[gtb])
        S.op("dve", lambda: nc.vector.tensor_tensor(out=a_v, in0=a_v, in1=gwv, op=ALU.add), reads=[gtb, gwb], writes=[gtb])
        S.op("dve", lambda: nc.vector.tensor_tensor(out=a_v, in0=a_v, in1=nea, op=ALU.mult), reads=[gtb, gparb], writes=[gtb])
        S.op("act", lambda: nc.scalar.activation(out=b_v, in_=b_v, func=AF.Sigmoid), reads=[gtb], writes=[gtb])
        S.op("sp", lambda r0=r0, nt=nt: nc.sync.dma_start(
            out=Sx.glb[r0:r0 + nt * 128, :].rearrange("(t p) c -> p t c", p=128), in_=gt[:, 0:nt, :]),
            reads=[gtb], writes=[C.b_glb], dma=True)


def pass_b(C, l):
    nc, S, I, Sx, ls = C.nc, C.S, C.I, C.Sx, C.ls
    T, TL = C.T, C.TL
    sb = lambda name, shape, dt=F32: ls.enter_context(nc.sbuf_tensor(name + "_b%d" % l, list(shape), dt))
    PL = min(2048, TL)
    cw = sb("cw", [128, 12, 5]); cwb = Buf("cw")
    S.op("sp", lambda: nc.sync.dma_start(out=cw[:], in_=I.conv_w[l]), writes=[cwb], dma=True)
    NB = 2
    xin = [sb("xin%d" % i, [128, PL + 4]) for i in range(NB)]; xinb = [Buf("xin%d" % i) for i in range(NB)]
    acc = [sb("acc%d" % i, [128, PL]) for i in range(NB)]; accb = [Buf("acc%d" % i) for i in range(NB)]
    yv = [sb("yv%d" % i, [128, PL]) for i in range(NB)]; yvb = [Buf("yv%d" % i) for i in range(NB)]
    sq = sb("sq", [128, PL]); sqb = Buf("sq")
    rs = [sb("rs%d" % i, [128, 512]) for i in range(2)]; rsb = [Buf("rs%d" % i) for i in range(2)]
    tk = [sb("tk%d" % i, [128, 4, 128]) for i in range(2)]; tkb = [Buf("tk%d" % i) for i in range(2)]
    C.b_qkn = Buf("s_qkn"); C.b_ktok = Buf("s_ktok"); C.b_vtok = Buf("s_vtok")
    segs = [(0, TC, 0, TC)]
    r = TC
    while r < T:
        segs.append((r, min(PL, T - r), TC, T))
        r += PL
    it = 0
    rsi = 0
    tki = 0
    for j in range(12):
        for (r0, L, s0, s1) in segs:
            b = it % NB
            it += 1
            lo = max(s0, r0 - 2)
            hi = min(s1, r0 + L + 2)
            if lo > r0 - 2:
                S.op("dve", lambda b=b: nc.vector.memset(xin[b][:, 0:2], 0.0), writes=[xinb[b]])
            if hi < r0 + L + 2:
                S.op("dve", lambda b=b, L=L: nc.vector.memset(xin[b][:, L + 2:L + 4], 0.0), writes=[xinb[b]])
            S.op("sp", lambda b=b, lo=lo, hi=hi, r0=r0: nc.sync.dma_start(
                out=xin[b][:, lo - (r0 - 2):hi - (r0 - 2)], in_=Sx.qkvT[j, :, lo:hi]),
                reads=[C.b_qkvT], writes=[xinb[b]], dma=True)
            S.op("dve", lambda b=b, L=L: nc.vector.tensor_scalar_mul(out=acc[b][:, 0:L], in0=xin[b][:, 0:L], scalar1=cw[:, j, 0:1]),
                 reads=[xinb[b], cwb], writes=[accb[b]])
            for tap in range(1, 5):
                S.op("dve", lambda b=b, L=L, tap=tap: nc.vector.scalar_tensor_tensor(
                    out=acc[b][:, 0:L], in0=xin[b][:, tap:tap + L], scalar=cw[:, j, tap:tap + 1], in1=acc[b][:, 0:L],
                    op0=ALU.mult, op1=ALU.add), reads=[xinb[b], cwb, accb[b]], writes=[accb[b]])
            S.op("act", lambda b=b, L=L: nc.scalar.activation(out=yv[b][:, 0:L], in_=acc[b][:, 0:L], func=AF.Silu),
                 reads=[accb[b]], writes=[yvb[b]])
            if j < 8:
                S.op("act", lambda b=b, L=L: nc.scalar.activation(out=sq[:, 0:L], in_=yv[b][:, 0:L], func=AF.Square),
                     reads=[yvb[b]], writes=[sqb])
                for c0 in range(0, L, 512):
                    n = min(512, L - c0)
                    pb = 0 + (c0 // 512) % 2
                    ri = rsi % 2
                    rsi += 1
                    S.op("pe", lambda c0=c0, n=n, pb=pb: nc.tensor.matmul(C.ps[pb][:, 0:n], C.cst[:, 1, :], sq[:, c0:c0 + n],
                                                                          start=True, stop=True),
                         reads=[sqb, C.cstb], writes=[C.psb[pb]])
                    S.op("act", lambda n=n, pb=pb, ri=ri: nc.scalar.activation(
                        out=rs[ri][:, 0:n], in_=C.ps[pb][:, 0:n], func=AF.Sqrt, bias=C.epsc[:, 0:1], scale=1.0),
                        reads=[C.psb[pb], C.cstb], writes=[rsb[ri]])
                    S.op("dve", lambda n=n, ri=ri: nc.vector.reciprocal(out=rs[ri][:, 0:n], in_=rs[ri][:, 0:n]),
                         reads=[rsb[ri]], writes=[rsb[ri]])
                    qs = (128.0 ** -0.5) if j < 4 else 1.0
                    S.op("dve", lambda b=b, c0=c0, n=n, ri=ri, qs=qs: nc.vector.scalar_tensor_tensor(
                        out=yv[b][:, c0:c0 + n], in0=yv[b][:, c0:c0 + n], scalar=qs, in1=rs[ri][:, 0:n],
                        op0=ALU.mult, op1=ALU.mult), reads=[yvb[b], rsb[ri]], writes=[yvb[b]])
                S.op("sp", lambda b=b, L=L, r0=r0: nc.sync.dma_start(out=Sx.qkn[j, :, r0:r0 + L], in_=yv[b][:, 0:L]),
                     reads=[yvb[b]], writes=[C.b_qkn], dma=True)
            if j >= 4:
                dst, dstb = (Sx.ktok, C.b_ktok) if j < 8 else (Sx.vtok, C.b_vtok)
                h = j % 4
                for c0 in range(0, L, 512):
                    n = min(512, L - c0)
                    nt = n // 128
                    pb = 2 + (c0 // 512) % 2
                    ti = tki % 2
                    tki += 1
                    for t in range(nt):
                        S.op("pe", lambda b=b, c0=c0, t=t, pb=pb: nc.tensor.transpose(
                            C.ps[pb][:, t * 128:(t + 1) * 128], yv[b][:, c0 + t * 128:c0 + (t + 1) * 128], C.cst[:, 0, :]),
                            reads=[yvb[b], C.cstb], writes=[C.psb[pb]])
                    S.op("dve", lambda ti=ti, pb=pb, n=n: nc.vector.tensor_copy(
                        out=tk[ti][:].rearrange("p t d -> p (t d)")[:, 0:n], in_=C.ps[pb][:, 0:n]),
                        reads=[C.psb[pb]], writes=[tkb[ti]])
                    S.op("sp", lambda ti=ti, r0=r0, c0=c0, nt=nt, n=n, h=h, dst=dst: nc.sync.dma_start(
                        out=dst[r0 + c0:r0 + c0 + n, h * 128:(h + 1) * 128].rearrange("(t p) d -> p t d", p=128),
                        in_=tk[ti][:, 0:nt, :]), reads=[tkb[ti]], writes=[dstb], dma=True)


def pass_cd(C, l):
    nc, S, I, Sx, ls = C.nc, C.S, C.I, C.Sx, C.ls
    T, NCH = C.T, C.NCH
    sb = lambda name, shape, dt=F32: ls.enter_context(nc.sbuf_tensor(name + "_c%d" % l, list(shape), dt))
    DT = F32
    cst = C.cst
    ident64 = cst[0:64, 0, 0:64]
    ones64 = cst[0:64, 1, :]
    tri = [cst[0:64, 2, 0:64], cst[0:64, 3, 0:64]]
    maskI = [cst[0:64, 2, 0:64], cst[0:64, 3, 0:64]]
    maskS = [cst[0:64, 4, 0:64], cst[0:64, 5, 0:64]]
    NB = 2
    qk = [[sb("qk%d_%d" % (d, i), [128, 8, 256]) for i in range(NB)] for d in range(2)]
    qkb = [[Buf("qk%d_%d" % (d, i)) for i in range(NB)] for d in range(2)]
    kt = [[sb("kt%d_%d" % (d, i), [64, 4, 512]) for i in range(NB)] for d in range(2)]
    ktb = [[Buf("kt%d_%d" % (d, i)) for i in range(NB)] for d in range(2)]
    vt = [[sb("vt%d_%d" % (d, i), [64, 4, 512]) for i in range(NB)] for d in range(2)]
    vtb = [[Buf("vt%d_%d" % (d, i)) for i in range(NB)] for d in range(2)]
    gl = [[sb("gl%d_%d" % (d, i), [64, 4, 16]) for i in range(NB)] for d in range(2)]
    glb_ = [[Buf("gl%d_%d" % (d, i)) for i in range(NB)] for d in range(2)]
    St = sb("St", [128, 8, 128]); Stb = Buf("St")
    S.op("dve", lambda: nc.vector.memset(St[:], 0.0), writes=[Stb])
    lgB = sb("lgB", [64, 8, 128]); lgBb = Buf("lgB")
    gcol = sb("gcol", [64, 8]); gcolb = Buf("gcol")
    gcB = sb("gcB", [128, 8]); gcBb = Buf("gcB")
    e1 = sb("e1", [64, 8]); e1b = Buf("e1")
    egc = sb("egc", [64, 8]); egcb = Buf("egc")
    bet = sb("bet", [64, 8]); betb = Buf("bet")
    dmat = sb("dmat", [64, 8, 64]); dmatb = Buf("dmat")
    E12 = sb("E12", [64, 2, 8, 64]); E12b = Buf("E12")
    E12m = sb("E12m", [64, 2, 8, 64]); E12mb = Buf("E12m")
    expG = sb("expG", [128, 8, 64]); expGb = Buf("expG")
    qg = sb("qg", [128, 8, 64], DT); qgb = Buf("qg")
    kd = sb("kd", [64, 8, 128], DT); kdb = Buf("kd")
    bek = sb("bek", [64, 8, 128]); bekb = Buf("bek")
    bv = sb("bv", [64, 8, 128]); bvb = Buf("bv")
    Lp = [sb("Lp%d" % i, [64, 8, 64]) for i in range(2)]; Lpb = [Buf("Lp%d" % i) for i in range(2)]
    Ap = [sb("Ap%d" % i, [64, 8, 64]) for i in range(2)]; Apb = [Buf("Ap%d" % i) for i in range(2)]
    qkm = sb("qkm", [64, 8, 64], DT); qkmb = Buf("qkm")
    Pm = sb("Pm", [64, 8, 64]); Pmb = Buf("Pm")
    usb = sb("usb", [64, 8, 128]); usbb = Buf("usb")
    wkT = sb("wkT", [128, 8, 64], DT); wkTb = Buf("wkT")
    Wsb = sb("Wsb", [64, 8, 128], DT); Wsbb = Buf("Wsb")
    osb = [sb("osb%d" % i, [64, 8, 128]) for i in range(4)]; osbb = [Buf("osb%d" % i) for i in range(4)]
    C.b_of = Buf("s_of"); C.b_ob = Buf("s_ob")
    E2s = sb("E2s", [64, 8, 64]); E2sb = Buf("E2s")
    dbet = sb("dbet", [64, 8, 64]); dbetb = Buf("dbet")
    identB = sb("identB", [64, 8, 64]); identBb = Buf("identB")
    for blk in range(8):
        S.op("dve", lambda blk=blk: nc.vector.tensor_copy(identB[:, blk, :], ident64), reads=[C.cstb], writes=[identBb])
    ps, psb = C.ps, C.psb
    P64 = lambda bank: ps[bank][0:64, :].rearrange("p (b c) -> p b c", b=8)

    import os
    lim = int(os.environ.get("CD_STAGE", "99"))
    ngroups = min(NCH // 4, int(os.environ.get("CD_GROUPS", "9999")))
    only = os.environ.get("CD_ONLY")
    for gI in range(ngroups):
        if only and str(gI) not in only.split(","):
            continue
        b = gI % NB
        r1 = gI * 256
        ct = (3 if gI == 0 else NCH + 3 - 4 * gI)
        r2 = (ct - 3) * 64
        for d, r in ((0, r1), (1, r2)):
            S.op("sp", lambda d=d, r=r, b=b: nc.sync.dma_start(
                out=qk[d][b][:], in_=Sx.qkn[:, :, r:r + 256].rearrange("j p t -> p j t")),
                reads=[C.b_qkn], writes=[qkb[d][b]], dma=True)
            S.op("sp", lambda d=d, r=r, b=b: nc.sync.dma_start(
                out=kt[d][b][:], in_=Sx.ktok[r:r + 256, :].rearrange("(n p) f -> p n f", p=64)),
                reads=[C.b_ktok], writes=[ktb[d][b]], dma=True)
            S.op("sp", lambda d=d, r=r, b=b: nc.sync.dma_start(
                out=vt[d][b][:], in_=Sx.vtok[r:r + 256, :].rearrange("(n p) f -> p n f", p=64)),
                reads=[C.b_vtok], writes=[vtb[d][b]], dma=True)
            S.op("sp", lambda d=d, r=r, b=b: nc.sync.dma_start(
                out=gl[d][b][:], in_=Sx.glb[r:r + 256, :].rearrange("(n p) f -> p n f", p=64)),
                reads=[C.b_glb], writes=[glb_[d][b]], dma=True)
        for si in range(min(4, int(os.environ.get("CD_STEPS", "4")))):
            ci = [si, 3 - si]
            rows = [r1 + si * 64, r2 + (3 - si) * 64]
            lgv = [gl[d][b][:, ci[d], d * 8:d * 8 + 4] for d in range(2)]
            btv = [gl[d][b][:, ci[d], d * 8 + 4:d * 8 + 8] for d in range(2)]
            qT = [[qk[d][b][:, h, ci[d] * 64:(ci[d] + 1) * 64] for h in range(4)] for d in range(2)]
            kT = [[qk[d][b][:, 4 + h, ci[d] * 64:(ci[d] + 1) * 64] for h in range(4)] for d in range(2)]
            ktv = [kt[d][b][:, ci[d], :].rearrange("p (h e) -> p h e", h=4) for d in range(2)]
            vtv = [vt[d][b][:, ci[d], :].rearrange("p (h e) -> p h e", h=4) for d in range(2)]
            for d in range(2):
                S.op("dve", lambda d=d: nc.vector.tensor_copy(lgB[:, d * 4:(d + 1) * 4, :], bc(lgv[d].unsqueeze(2), [64, 4, 128])),
                     reads=[glb_[d][b]], writes=[lgBb])
                S.op("dve", lambda d=d: nc.vector.tensor_copy(bet[:, d * 4:(d + 1) * 4], btv[d]), reads=[glb_[d][b]], writes=[betb])
            for d in range(2):
                for h in range(4):
                    blk = d * 4 + h
                    S.op("pe", lambda d=d, blk=blk: nc.tensor.matmul(ps[0][:, blk * 64:(blk + 1) * 64], lgB[:, blk, :], tri[d],
                                                                     start=True, stop=True),
                         reads=[lgBb, C.cstb], writes=[psb[0]])
                S.op("pe", lambda d=d: nc.tensor.matmul(ps[1][0:64, d * 16:d * 16 + 4], tri[d], lgv[d], start=True, stop=True),
                     reads=[glb_[d][b], C.cstb], writes=[psb[1]])
                S.op("pe", lambda d=d: nc.tensor.matmul(ps[1][:, d * 16 + 8:d * 16 + 12], ones64, lgv[d], start=True, stop=True),
                     reads=[glb_[d][b], C.cstb], writes=[psb[1]])
            p1v = ps[1][:, 0:32].rearrange("p (d x) -> p d x", d=2)
            if lim < 3:
                continue
            S.op("dve", lambda: nc.vector.tensor_copy(gcol[:].rearrange("p (d h) -> p d h", d=2), p1v[0:64, :, 0:4]),
                 reads=[psb[1]], writes=[gcolb])
            S.op("act", lambda: nc.scalar.activation(out=gcB[:].rearrange("p (d h) -> p d h", d=2), in_=p1v[:, :, 8:12], func=AF.Exp),
                 reads=[psb[1]], writes=[gcBb])
            S.op("dve", lambda: nc.vector.tensor_tensor(out=e1[:].rearrange("p (d h) -> p d h", d=2), in0=p1v[0:64, :, 8:12],
                                                        in1=gcol[:].rearrange("p (d h) -> p d h", d=2), op=ALU.subtract),
                 reads=[psb[1], gcolb], writes=[e1b])
            S.op("act", lambda: nc.scalar.activation(out=e1[:], in_=e1[:], func=AF.Exp), reads=[e1b], writes=[e1b])
            S.op("act", lambda: nc.scalar.activation(out=egc[:], in_=gcol[:], func=AF.Exp), reads=[gcolb], writes=[egcb])
            S.op("dve", lambda: nc.vector.tensor_tensor(out=egc[:], in0=egc[:], in1=bet[:], op=ALU.mult),
                 reads=[egcb, betb], writes=[egcb])
            if lim < 4:
                continue
            S.op("dve", lambda: nc.vector.tensor_tensor(out=dmat[:], in0=P64(0), in1=bc(gcol[:].unsqueeze(2), [64, 8, 64]),
                                                        op=ALU.subtract), reads=[psb[0], gcolb], writes=[dmatb])
            S.op("dve", lambda: nc.vector.tensor_scalar(out=E12[:, 0, :, :], in0=dmat[:], scalar1=-1.0, scalar2=0.0,
                                                        op0=ALU.mult, op1=ALU.min), reads=[dmatb], writes=[E12b])
            S.op("dve", lambda: nc.vector.tensor_scalar_min(out=E12[:, 1, :, :], in0=dmat[:], scalar1=0.0),
                 reads=[dmatb], writes=[E12b])
            S.op("act", lambda: nc.scalar.activation(out=E12[:], in_=E12[:], func=AF.Exp), reads=[E12b], writes=[E12b])
            S.op("act", lambda: nc.scalar.activation(out=expG[:], in_=ps[0][:, :].rearrange("p (b c) -> p b c", b=8), func=AF.Exp),
                 reads=[psb[0]], writes=[expGb])
            for d in range(2):
                S.op("pool", lambda d=d: nc.gpsimd.tensor_tensor(
                    out=E12m[:, 0, d * 4:(d + 1) * 4, :], in0=E12[:, 0, d * 4:(d + 1) * 4, :],
                    in1=bc(maskS[d].unsqueeze(1), [64, 4, 64]), op=ALU.mult), reads=[E12b, C.cstb], writes=[E12mb])
                S.op("pool", lambda d=d: nc.gpsimd.tensor_tensor(
                    out=E12m[:, 1, d * 4:(d + 1) * 4, :], in0=E12[:, 1, d * 4:(d + 1) * 4, :],
                    in1=bc(maskI[d].unsqueeze(1), [64, 4, 64]), op=ALU.mult), reads=[E12b, C.cstb], writes=[E12mb])
                S.op("pool", lambda d=d: nc.gpsimd.tensor_tensor(
                    out=E2s[:, d * 4:(d + 1) * 4, :], in0=E12[:, 1, d * 4:(d + 1) * 4, :],
                    in1=bc(maskS[1 - d].unsqueeze(1), [64, 4, 64]), op=ALU.mult), reads=[E12b, C.cstb], writes=[E2sb])
                S.op("dve", lambda d=d: nc.vector.tensor_tensor(
                    out=qg[:, d * 4:(d + 1) * 4, :], in0=qk[d][b][:, 0:4, ci[d] * 64:(ci[d] + 1) * 64],
                    in1=expG[:, d * 4:(d + 1) * 4, :], op=ALU.mult), reads=[qkb[d][b], expGb], writes=[qgb])
                S.op("dve", lambda d=d: nc.vector.tensor_tensor(
                    out=kd[:, d * 4:(d + 1) * 4, :], in0=ktv[d], in1=bc(e1[:, d * 4:(d + 1) * 4].unsqueeze(2), [64, 4, 128]),
                    op=ALU.mult), reads=[ktb[d][b], e1b], writes=[kdb])
                S.op("pool", lambda d=d: nc.gpsimd.tensor_tensor(
                    out=bek[:, d * 4:(d + 1) * 4, :], in0=ktv[d], in1=bc(egc[:, d * 4:(d + 1) * 4].unsqueeze(2), [64, 4, 128]),
                    op=ALU.mult), reads=[ktb[d][b], egcb], writes=[bekb])
                S.op("pool", lambda d=d: nc.gpsimd.tensor_tensor(
                    out=bv[:, d * 4:(d + 1) * 4, :], in0=vtv[d], in1=bc(bet[:, d * 4:(d + 1) * 4].unsqueeze(2), [64, 4, 128]),
                    op=ALU.mult), reads=[vtb[d][b], betb], writes=[bvb])
            if lim < 6:
                continue
            for d in range(2):
                for h in range(4):
                    blk = d * 4 + h
                    S.op("pe", lambda d=d, h=h, blk=blk: nc.tensor.matmul(ps[2][0:64, blk * 64:(blk + 1) * 64], kT[d][h], kT[d][h],
                                                                          start=True, stop=True),
                         reads=[qkb[d][b]], writes=[psb[2]])
                    S.op("pe", lambda d=d, h=h, blk=blk: nc.tensor.matmul(ps[3][0:64, blk * 64:(blk + 1) * 64], kT[d][h], qT[d][h],
                                                                          start=True, stop=True),
                         reads=[qkb[d][b]], writes=[psb[3]])
            if lim < 7:
                continue
            S.op("dve", lambda: nc.vector.tensor_tensor(out=Lp[0][:], in0=P64(2), in1=E12m[:, 0, :, :], op=ALU.mult),
                 reads=[psb[2], E12mb], writes=[Lpb[0]])
            S.op("dve", lambda: nc.vector.tensor_tensor(out=Lp[0][:], in0=Lp[0][:], in1=bc(bet[:].unsqueeze(2), [64, 8, 64]),
                                                        op=ALU.mult), reads=[Lpb[0], betb], writes=[Lpb[0]])
            S.op("dve", lambda: nc.vector.tensor_tensor(out=qkm[:], in0=P64(3), in1=E12m[:, 1, :, :], op=ALU.mult),
                 reads=[psb[3], E12mb], writes=[qkmb])
            if lim < 8:
                continue
            S.op("dve", lambda: nc.vector.tensor_tensor(out=dbet[:], in0=identB[:], in1=bc(bet[:].unsqueeze(2), [64, 8, 64]),
                                                        op=ALU.mult), reads=[betb, identBb], writes=[dbetb])
            for blk in range(8):
                S.op("pe", lambda blk=blk: nc.tensor.matmul(ps[7][0:64, blk * 64:(blk + 1) * 64], cst[0:64, 1, 0:64], dbet[:, blk, :],
                                                            start=True, stop=True), reads=[dbetb, C.cstb], writes=[psb[7]])
            S.op("dve", lambda: nc.vector.tensor_tensor(out=Ap[0][:], in0=P64(2), in1=E2s[:], op=ALU.mult),
                 reads=[psb[2], E2sb], writes=[Apb[0]])
            S.op("dve", lambda: nc.vector.tensor_tensor(out=Ap[0][:], in0=Ap[0][:], in1=P64(7), op=ALU.mult),
                 reads=[psb[7], Apb[0]], writes=[Apb[0]])
            S.op("pool", lambda: nc.gpsimd.tensor_tensor(out=Pm[:], in0=identB[:], in1=Ap[0][:], op=ALU.subtract),
                 reads=[Apb[0], identBb], writes=[Pmb])
            if lim < 9:
                continue
            for j in range(1, 6):
                cur, prv = j % 2, (j - 1) % 2
                for blk in range(8):
                    S.op("pe", lambda blk=blk, prv=prv: nc.tensor.matmul(ps[5][0:64, blk * 64:(blk + 1) * 64], Ap[prv][:, blk, :],
                                                                         Lp[prv][:, blk, :], start=True, stop=True),
                         reads=[Apb[prv], Lpb[prv]], writes=[psb[5]])
                if j <= 4:
                    for blk in range(8):
                        S.op("pe", lambda blk=blk, prv=prv: nc.tensor.matmul(ps[6][0:64, blk * 64:(blk + 1) * 64], Lp[prv][:, blk, :],
                                                                             Ap[prv][:, blk, :], start=True, stop=True),
                             reads=[Apb[prv], Lpb[prv]], writes=[psb[6]])
                S.op("dve", lambda cur=cur: nc.vector.tensor_copy(out=Lp[cur][:], in_=P64(5)), reads=[psb[5]], writes=[Lpb[cur]])
                if j <= 4:
                    S.op("dve", lambda cur=cur: nc.vector.tensor_copy(Ap[cur][:], P64(6)), reads=[psb[6]], writes=[Apb[cur]])
                for blk in range(8):
                    S.op("pe", lambda blk=blk, cur=cur: nc.tensor.matmul(ps[7][0:64, blk * 64:(blk + 1) * 64], Lp[cur][:, blk, :],
                                                                         Pm[:, blk, :], start=True, stop=True),
                         reads=[Lpb[cur], Pmb], writes=[psb[7]])
                S.op("dve", lambda: nc.vector.tensor_tensor(out=Pm[:], in0=Pm[:], in1=P64(7), op=ALU.add),
                     reads=[Pmb, psb[7]], writes=[Pmb])
            if lim < 10:
                continue
            for d in range(2):
                for h in range(4):
                    blk = d * 4 + h
                    S.op("pe", lambda d=d, h=h, blk=blk: nc.tensor.matmul(ps[2 + d][0:64, h * 128:(h + 1) * 128], Pm[:, blk, :],
                                                                          bv[:, blk, :], start=True, stop=True),
                         reads=[Pmb, bvb], writes=[psb[2 + d]])
                    S.op("pe", lambda blk=blk: nc.tensor.matmul(ps[4][:, blk * 64:(blk + 1) * 64], bek[:, blk, :], Pm[:, blk, :],
                                                                start=True, stop=True),
                         reads=[Pmb, bekb], writes=[psb[4]])
            for d in range(2):
                S.op("dve", lambda d=d: nc.vector.tensor_copy(out=usb[:, d * 4:(d + 1) * 4, :].rearrange("p b e -> p (b e)"),
                                                       in_=ps[2 + d][0:64, :]), reads=[psb[2 + d]], writes=[usbb])
            S.op("dve", lambda: nc.vector.tensor_copy(wkT[:].rearrange("p b c -> p (b c)"), ps[4][:, :]),
                 reads=[psb[4]], writes=[wkTb])
            if lim < 11:
                continue
            for d in range(2):
                for h in range(4):
                    blk = d * 4 + h
                    S.op("pe", lambda d=d, h=h, blk=blk: nc.tensor.matmul(ps[5 + d][0:64, h * 128:(h + 1) * 128], wkT[:, blk, :],
                                                                          St[:, blk, :], start=True, stop=True),
                         reads=[wkTb, Stb], writes=[psb[5 + d]])
            for d in range(2):
                S.op("dve", lambda d=d: nc.vector.tensor_tensor(
                    out=Wsb[:, d * 4:(d + 1) * 4, :].rearrange("p b e -> p (b e)"),
                    in0=usb[:, d * 4:(d + 1) * 4, :].rearrange("p b e -> p (b e)"), in1=ps[5 + d][0:64, :], op=ALU.subtract),
                    reads=[usbb, psb[5 + d]], writes=[Wsbb])
            sub = int(os.environ.get("CD_SUB", "9"))
            if sub < 2:
                continue
            ob_ = (0, 7)
            for d in range(2):
                for h in range(4):
                    blk = d * 4 + h
                    S.op("pe", lambda d=d, h=h, blk=blk: nc.tensor.matmul(ps[ob_[d]][0:64, h * 128:(h + 1) * 128], qg[:, blk, :],
                                                                          St[:, blk, :], start=True, stop=True),
                         reads=[qgb, Stb], writes=[psb[ob_[d]]])
                    S.op("pe", lambda d=d, h=h, blk=blk: nc.tensor.matmul(ps[5 + d][0:64, h * 128:(h + 1) * 128], qkm[:, blk, :],
                                                                          Wsb[:, blk, :], start=True, stop=True),
                         reads=[qkmb, Wsbb], writes=[psb[5 + d]])
            oi = si
            for d in range(2):
                S.op("dve", lambda d=d, oi=oi: nc.vector.tensor_copy(out=osb[oi][:, d * 4:(d + 1) * 4, :].rearrange("p b e -> p (b e)"),
                                                              in_=ps[ob_[d]][0:64, :]), reads=[psb[ob_[d]]], writes=[osbb[oi]])
                S.op("dve", lambda d=d, oi=oi: nc.vector.tensor_tensor(
                    out=osb[oi][:, d * 4:(d + 1) * 4, :].rearrange("p b e -> p (b e)"),
                    in0=osb[oi][:, d * 4:(d + 1) * 4, :].rearrange("p b e -> p (b e)"), in1=ps[5 + d][0:64, :], op=ALU.add),
                    reads=[psb[5 + d], osbb[oi]], writes=[osbb[oi]])
                dst, dstb = (Sx.of, C.b_of) if d == 0 else (Sx.ob, C.b_ob)
                S.op("sp", lambda d=d, oi=oi, dst=dst: nc.sync.dma_start(
                    out=dst[rows[d]:rows[d] + 64, :], in_=osb[oi][:, d * 4:(d + 1) * 4, :].rearrange("p b e -> p (b e)")),
                    reads=[osbb[oi]], writes=[dstb], dma=True)
            if sub < 3:
                continue
            for d in range(2):
                for h in range(4):
                    blk = d * 4 + h
                    S.op("pe", lambda d=d, h=h, blk=blk: nc.tensor.matmul(ps[2 + d][:, h * 128:(h + 1) * 128], kd[:, blk, :],
                                                                          Wsb[:, blk, :], start=True, stop=True),
                         reads=[kdb, Wsbb], writes=[psb[2 + d]])
            if sub < 4:
                continue
            for d in range(2):
                for h in range(4):
                    blk = d * 4 + h
                    S.op("dve", lambda d=d, h=h, blk=blk: nc.vector.scalar_tensor_tensor(
                        out=St[:, blk, :], in0=St[:, blk, :], scalar=gcB[:, blk:blk + 1], in1=ps[2 + d][:, h * 128:(h + 1) * 128],
                        op0=ALU.mult, op1=ALU.add), reads=[Stb, gcBb, psb[2 + d]], writes=[Stb])


def pass_e(C, l):
    nc, S, I, Sx, ls = C.nc, C.S, C.I, C.Sx, C.ls
    T = C.T
    sb = lambda name, shape, dt=F32: ls.enter_context(nc.sbuf_tensor(name + "_e%d" % l, list(shape), dt))
    ps, psb = C.ps, C.psb
    xsrc = I.x if l == 0 else Sx.x1
    xsrcb = Buf("xin_e") if l == 0 else C.b_x1
    wpa = sb("wpa", [128, 4, D], BF16); wpab = Buf("wpa")
    wpb = sb("wpb", [128, 4, D], BF16); wpbb = Buf("wpb")
    wo = sb("wo", [128, 8, D], BF16); wob = Buf("wo")
    S.op("pool", lambda: nc.gpsimd.dma_start(out=wpa[:], in_=I.w_pa[l].rearrange("(c p) d -> p c d", p=128)), writes=[wpab], dma=True)
    S.op("pool", lambda: nc.gpsimd.dma_start(out=wpb[:], in_=I.w_pb[l].rearrange("(c p) d -> p c d", p=128)), writes=[wpbb], dma=True)
    S.op("pool", lambda: nc.gpsimd.dma_start(out=wo[:], in_=I.w_o[l].rearrange("(c p) d -> p c d", p=128)), writes=[wob], dma=True)
    lnp = sb("lnp", [128, 4, D]); lnpb = Buf("lnp")
    S.op("sp", lambda: nc.sync.dma_start(out=lnp[:], in_=I.lnp[l]), writes=[lnpb], dma=True)
    nw = sb("nw", [128, 512]); nwb = Buf("nw")
    S.op("sp", lambda: nc.sync.dma_start(out=nw[:], in_=I.gdn_nw[l]), writes=[nwb], dma=True)
    moe = (l == 1)
    if moe:
        rt = sb("rt", [128, 8, NE]); rtb = Buf("rt")
        S.op("sp", lambda: nc.sync.dma_start(out=rt[:], in_=I.router[:, :, :]), writes=[rtb], dma=True)
        h32 = sb("h32", [128, 8, 512]); h32b = Buf("h32")
        lg8 = sb("lg8", [128, 4, NE]); lg8b = Buf("lg8")
        lgx = sb("lgx", [128, 4, NE]); lgxb = Buf("lgx")
        eq = sb("eq", [128, 2, 4, NE]); eqb = Buf("eq")
        mm = sb("mm", [128, 4, 4]); mmb = Buf("mm")
        gate = sb("gate", [128, 4, NE]); gateb = Buf("gate")
    tA = sb("tA", [128, 512]); tAb = Buf("tA")
    tB = sb("tB", [128, 512]); tBb = Buf("tB")
    tZ = sb("tZ", [128, 512]); tZb = Buf("tZ")
    ss = sb("ss", [128, 8]); ssb = Buf("ss")
    goT = sb("goT", [128, 4, 512], BF16); goTb = Buf("goT")
    oaT = sb("oaT", [128, 4, 512], BF16); oaTb = Buf("oaT")
    sg = [sb("sg%d" % i, [128, 2, 512], BF16) for i in range(2)]; sgb = [Buf("sg%d" % i) for i in range(2)]
    yT = sb("yT", [128, 8, 512], BF16); yTb = Buf("yT")
    xt = sb("xt", [128, D]); xtb = Buf("xt")
    rr = sb("rr", [128, D]); rrb = Buf("rr")
    x1t = sb("x1t", [128, 4, D]); x1tb = Buf("x1t")
    xn = sb("xn", [128, D]); xnb = Buf("xn")
    st6 = sb("st6", [128, 3, 6]); stb = Buf("st6")
    h2T = sb("h2T", [128, 8, 512], BF16); h2Tb = Buf("h2T")
    facc = sb("facc", [128, 4, D]); faccb = Buf("facc")
    NWB = 2
    w1b = [sb("w1b%d" % i, [128, 8, 256], BF16) for i in range(NWB)]
    w3b = [sb("w3b%d" % i, [128, 8, 256], BF16) for i in range(NWB)]
    w2b = [sb("w2b%d" % i, [128, 2, D], BF16) for i in range(NWB)]
    w1bb = [Buf("wblk1_%d" % i) for i in range(NWB)]
    w3bb = [Buf("wblk3_%d" % i) for i in range(NWB)]
    w2bb = [Buf("wblk2_%d" % i) for i in range(NWB)]
    sa = [sb("sa%d" % i, [128, 512], BF16) for i in range(2)]; sab = [Buf("sa%d" % i) for i in range(2)]
    gT = [sb("gT%d" % i, [128, 2, 512], BF16) for i in range(2)]; gTb = [Buf("gT%d" % i) for i in range(2)]
    own_hi = TC + C.OWN
    dstb = C.b_x1 if l == 0 else Buf("yout")
    C.b_x1 = getattr(C, "b_x1", Buf("s_x1"))
    if l == 0:
        dstb = C.b_x1
    wit = 0
    for gi, (r0, nt, isctx) in enumerate(groups_of(C, 4)):
        if l == 1 and (isctx or r0 >= own_hi):
            continue
        N = nt * 128
        for t in range(nt):
            rows = slice(r0 + t * 128, r0 + (t + 1) * 128)
            S.op("sp", lambda rows=rows: nc.sync.dma_start(out=tA[:], in_=Sx.of[rows, :]), reads=[C.b_of], writes=[tAb], dma=True)
            S.op("sp", lambda rows=rows: nc.sync.dma_start(out=tB[:], in_=Sx.ob[rows, :]), reads=[C.b_ob], writes=[tBb], dma=True)
            S.op("sp", lambda rows=rows: nc.sync.dma_start(out=tZ[:], in_=Sx.zs[rows, :]), reads=[C.b_zs], writes=[tZb], dma=True)
            S.op("dve", lambda: nc.vector.tensor_tensor(out=tA[:], in0=tA[:], in1=tB[:], op=ALU.add), reads=[tAb, tBb], writes=[tAb])
            S.op("dve", lambda: nc.vector.tensor_tensor(out=tB[:], in0=tA[:], in1=tA[:], op=ALU.mult), reads=[tAb], writes=[tBb])
            S.op("dve", lambda: nc.vector.tensor_reduce(out=ss[:, 0:4], in_=tB[:].rearrange("p (h e) -> p h e", h=4), axis=AX.X, op=ALU.add),
                 reads=[tBb], writes=[ssb])
            S.op("act", lambda: nc.scalar.activation(out=ss[:, 4:8], in_=ss[:, 0:4], func=AF.Sqrt, bias=C.epsc[:, 0:1], scale=1.0 / 128.0),
                 reads=[ssb, C.cstb], writes=[ssb])
            S.op("dve", lambda: nc.vector.reciprocal(out=ss[:, 4:8], in_=ss[:, 4:8]), reads=[ssb], writes=[ssb])
            S.op("dve", lambda: nc.vector.tensor_tensor(out=tA[:].rearrange("p (h e) -> p h e", h=4), in0=tA[:].rearrange("p (h e) -> p h e", h=4),
                                                        in1=bc(ss[:, 4:8].unsqueeze(2), [128, 4, 128]), op=ALU.mult), reads=[tAb, ssb], writes=[tAb])
            S.op("dve", lambda: nc.vector.tensor_tensor(out=tA[:], in0=tA[:], in1=nw[:], op=ALU.mult), reads=[tAb, nwb], writes=[tAb])
            S.op("dve", lambda: nc.vector.tensor_tensor(out=tA[:], in0=tA[:], in1=tZ[:], op=ALU.mult), reads=[tAb, tZb], writes=[tAb])
            for h in range(4):
                S.op("pe", lambda h=h: nc.tensor.transpose(ps[6][:, h * 128:(h + 1) * 128], tA[:, h * 128:(h + 1) * 128], C.cst[:, 0, :]),
                     reads=[tAb, C.cstb], writes=[psb[6]])
            S.op("dve", lambda t=t: nc.vector.tensor_copy(out=goT[:, :, t * 128:(t + 1) * 128], in_=ps[6][:, :].rearrange("p (h e) -> p h e", h=4)),
                 reads=[psb[6]], writes=[goTb])
        S.op("sp", lambda r0=r0, N=N: nc.sync.dma_start(out=oaT[:, :, 0:N], in_=Sx.oaT[:, :, r0:r0 + N].rearrange("c p t -> p c t")),
             reads=[C.b_oaT], writes=[oaTb], dma=True)
        for m in range(8):
            si = m % 2
            S.op("sp", lambda si=si, m=m, r0=r0, N=N: nc.sync.dma_start(out=sg[si][:, 0, 0:N], in_=Sx.sgT[m, :, r0:r0 + N]),
                 reads=[C.b_sgT], writes=[sgb[si]], dma=True)
            S.op("sp", lambda si=si, m=m, r0=r0, N=N: nc.sync.dma_start(out=sg[si][:, 1, 0:N], in_=Sx.sgT[8 + m, :, r0:r0 + N]),
                 reads=[C.b_sgT], writes=[sgb[si]], dma=True)
            pa, pb_ = (m % 2) * 2, (m % 2) * 2 + 1
            for c in range(4):
                S.op("pe", lambda c=c, m=m, pa=pa, N=N: nc.tensor.matmul(ps[pa][:, 0:N], wpa[:, c, m * 128:(m + 1) * 128], oaT[:, c, 0:N],
                                                                         start=(c == 0), stop=(c == 3)), reads=[wpab, oaTb], writes=[psb[pa]])
            for c in range(4):
                S.op("pe", lambda c=c, m=m, pb_=pb_, N=N: nc.tensor.matmul(ps[pb_][:, 0:N], wpb[:, c, m * 128:(m + 1) * 128], goT[:, c, 0:N],
                                                                           start=(c == 0), stop=(c == 3)), reads=[wpbb, goTb], writes=[psb[pb_]])
            S.op("dve", lambda si=si, pa=pa, N=N: nc.vector.tensor_tensor(out=tB[:, 0:N], in0=ps[pa][:, 0:N], in1=sg[si][:, 0, 0:N], op=ALU.mult),
                 reads=[psb[pa], sgb[si]], writes=[tBb])
            S.op("dve", lambda si=si, pb_=pb_, N=N: nc.vector.tensor_tensor(out=tZ[:, 0:N], in0=ps[pb_][:, 0:N], in1=sg[si][:, 1, 0:N], op=ALU.mult),
                 reads=[psb[pb_], sgb[si]], writes=[tZb])
            S.op("pool", lambda m=m, N=N: nc.gpsimd.tensor_tensor(out=yT[:, m, 0:N], in0=tB[:, 0:N], in1=tZ[:, 0:N], op=ALU.add),
                 reads=[tBb, tZb], writes=[yTb])
        for t in range(nt):
            rows = slice(r0 + t * 128, r0 + (t + 1) * 128)
            S.op("sp", lambda rows=rows: nc.sync.dma_start(out=xt[:], in_=xsrc[rows, :]), reads=[xsrcb], writes=[xtb], dma=True)
            for hh in range(2):
                for k in range(8):
                    S.op("pe", lambda k=k, hh=hh, t=t: nc.tensor.matmul(ps[4 + hh][:, :], yT[:, k, t * 128:(t + 1) * 128],
                                                                        wo[:, k, hh * 512:(hh + 1) * 512], start=(k == 0), stop=(k == 7)),
                         reads=[yTb, wob], writes=[psb[4 + hh]])
                S.op("dve", lambda hh=hh: nc.vector.tensor_tensor(out=rr[:, hh * 512:(hh + 1) * 512], in0=ps[4 + hh][:, :],
                                                                  in1=C.mod_bc[:, 0, isctx, hh * 512:(hh + 1) * 512], op=ALU.mult),
                     reads=[psb[4 + hh], C.mod_bcb], writes=[rrb])
            S.op("dve", lambda: nc.vector.scalar_tensor_tensor(out=rr[:], in0=xt[:], scalar=ALPHA, in1=rr[:], op0=ALU.mult, op1=ALU.add),
                 reads=[xtb, rrb], writes=[rrb])
            ln_affine(C, rr, rrb, x1t[:, t, :], x1tb, lnp[:, 0, :], lnp[:, 1, :], lnpb, st6, stb)
        ln_modT(C, l, x1t, x1tb, nt, 3, isctx, h2T, h2Tb, "e", xn, xnb, st6, stb, hT32=((h32, h32b) if moe else None),
                use_act=False)
        if moe:
            for t in range(nt):
                for k in range(8):
                    S.op("pe", lambda k=k, t=t: nc.tensor.matmul(ps[6][:, 0:NE], h32[:, k, t * 128:(t + 1) * 128], rt[:, k, :],
                                                                 start=(k == 0), stop=(k == 7)), reads=[h32b, rtb], writes=[psb[6]])
                S.op("dve", lambda t=t: nc.vector.tensor_copy(lg8[:, t, :], ps[6][:, 0:NE]), reads=[psb[6]], writes=[lg8b])
                S.op("dve", lambda t=t: nc.vector.tensor_reduce(out=mm[:, t, 0:1], in_=lg8[:, t, :], axis=AX.X, op=ALU.max), reads=[lg8b], writes=[mmb])
                S.op("dve", lambda t=t: nc.vector.tensor_scalar(out=eq[:, 0, t, :], in0=lg8[:, t, :], scalar1=mm[:, t, 0:1], scalar2=None,
                                                                op0=ALU.is_equal), reads=[lg8b, mmb], writes=[eqb])
                S.op("dve", lambda t=t: nc.vector.scalar_tensor_tensor(out=lgx[:, t, :], in0=eq[:, 0, t, :], scalar=-1.0e30, in1=lg8[:, t, :],
                                                                       op0=ALU.mult, op1=ALU.add), reads=[eqb, lg8b], writes=[lgxb])
                S.op("dve", lambda t=t: nc.vector.tensor_reduce(out=mm[:, t, 1:2], in_=lgx[:, t, :], axis=AX.X, op=ALU.max), reads=[lgxb], writes=[mmb])
                S.op("dve", lambda t=t: nc.vector.tensor_scalar(out=eq[:, 1, t, :], in0=lgx[:, t, :], scalar1=mm[:, t, 1:2], scalar2=None,
                                                                op0=ALU.is_equal), reads=[lgxb, mmb], writes=[eqb])
                S.op("dve", lambda t=t: nc.vector.tensor_tensor(out=mm[:, t, 2:3], in0=mm[:, t, 0:1], in1=mm[:, t, 1:2], op=ALU.subtract),
                     reads=[mmb], writes=[mmb])
                S.op("act", lambda t=t: nc.scalar.activation(out=mm[:, t, 2:3], in_=mm[:, t, 2:3], func=AF.Tanh, scale=0.5), reads=[mmb], writes=[mmb])
                S.op("dve", lambda t=t: nc.vector.tensor_scalar(out=mm[:, t, 2:3], in0=mm[:, t, 2:3], scalar1=0.5, scalar2=0.5,
                                                                op0=ALU.mult, op1=ALU.add), reads=[mmb], writes=[mmb])
                S.op("dve", lambda t=t: nc.vector.tensor_scalar(out=mm[:, t, 3:4], in0=mm[:, t, 2:3], scalar1=-1.0, scalar2=1.0,
                                                                op0=ALU.mult, op1=ALU.add), reads=[mmb], writes=[mmb])
                S.op("dve", lambda t=t: nc.vector.tensor_scalar_mul(out=gate[:, t, :], in0=eq[:, 0, t, :], scalar1=mm[:, t, 2:3]),
                     reads=[eqb, mmb], writes=[gateb])
                S.op("dve", lambda t=t: nc.vector.scalar_tensor_tensor(out=gate[:, t, :], in0=eq[:, 1, t, :], scalar=mm[:, t, 3:4], in1=gate[:, t, :],
                                                                       op0=ALU.mult, op1=ALU.add), reads=[eqb, mmb, gateb], writes=[gateb])
        experts = list(range(1, C.ne + 1)) if moe else [0]
        first = True
        for e in experts:
            for jb in range(DFF // 256):
                wi = wit % NWB
                wit += 1
                c0 = jb * 256
                S.op("sp", lambda wi=wi, e=e, c0=c0: nc.sync.dma_start(
                    out=w1b[wi][:], in_=Sx.wb1[e][:, c0:c0 + 256].rearrange("(k p) c -> p k c", p=128)),
                    reads=[C.wb[e]], writes=[w1bb[wi]], dma=True)
                S.op("sp", lambda wi=wi, e=e, c0=c0: nc.sync.dma_start(
                    out=w3b[wi][:], in_=Sx.wb3[e][:, c0:c0 + 256].rearrange("(k p) c -> p k c", p=128)),
                    reads=[C.wb[e]], writes=[w3bb[wi]], dma=True)
                S.op("sp", lambda wi=wi, e=e, c0=c0: nc.sync.dma_start(
                    out=w2b[wi][:], in_=Sx.wb2[e][c0:c0 + 256, :].rearrange("(c p) d -> p c d", p=128)),
                    reads=[C.wb[e]], writes=[w2bb[wi]], dma=True)
                gi2 = jb % 2
                for c in range(2):
                    for k in range(8):
                        S.op("pe", lambda k=k, c=c, wi=wi, N=N: nc.tensor.matmul(ps[0 + 2 * c][:, 0:N], w1b[wi][:, k, c * 128:(c + 1) * 128],
                                                                                 h2T[:, k, 0:N], start=(k == 0), stop=(k == 7)),
                             reads=[w1bb[wi], h2Tb], writes=[psb[0 + 2 * c]])
                    for k in range(8):
                        S.op("pe", lambda k=k, c=c, wi=wi, N=N: nc.tensor.matmul(ps[1 + 2 * c][:, 0:N], w3b[wi][:, k, c * 128:(c + 1) * 128],
                                                                                 h2T[:, k, 0:N], start=(k == 0), stop=(k == 7)),
                             reads=[w3bb[wi], h2Tb], writes=[psb[1 + 2 * c]])
                    S.op("act", lambda c=c, N=N: nc.scalar.activation(out=sa[c][:, 0:N], in_=ps[0 + 2 * c][:, 0:N], func=AF.Silu),
                         reads=[psb[0 + 2 * c]], writes=[sab[c]])
                    S.op("dve", lambda c=c, gi2=gi2, N=N: nc.vector.tensor_tensor(out=gT[gi2][:, c, 0:N], in0=ps[1 + 2 * c][:, 0:N],
                                                                                  in1=sa[c][:, 0:N], op=ALU.mult),
                         reads=[psb[1 + 2 * c], sab[c]], writes=[gTb[gi2]])
                for t in range(nt):
                    for hh in range(2):
                        pbk = 4 + hh
                        for c in range(2):
                            S.op("pe", lambda c=c, t=t, hh=hh, wi=wi, gi2=gi2, pbk=pbk: nc.tensor.matmul(
                                ps[pbk][:, :], gT[gi2][:, c, t * 128:(t + 1) * 128], w2b[wi][:, c, hh * 512:(hh + 1) * 512],
                                start=(c == 0), stop=(c == 1)), reads=[gTb[gi2], w2bb[wi]], writes=[psb[pbk]])
                        fa = facc[:, t, hh * 512:(hh + 1) * 512]
                        if moe:
                            gs = gate[:, t, e - 1:e]
                            if first:
                                S.op("dve", lambda fa=fa, gs=gs, pbk=pbk: nc.vector.tensor_scalar_mul(out=fa, in0=ps[pbk][:, :], scalar1=gs),
                                     reads=[psb[pbk], gateb], writes=[faccb])
                            else:
                                S.op("dve", lambda fa=fa, gs=gs, pbk=pbk: nc.vector.scalar_tensor_tensor(
                                    out=fa, in0=ps[pbk][:, :], scalar=gs, in1=fa, op0=ALU.mult, op1=ALU.add),
                                    reads=[psb[pbk], gateb, faccb], writes=[faccb])
                        else:
                            if first:
                                S.op("dve", lambda fa=fa, pbk=pbk: nc.vector.tensor_copy(out=fa, in_=ps[pbk][:, :]), reads=[psb[pbk]], writes=[faccb])
                            else:
                                S.op("dve", lambda fa=fa, pbk=pbk: nc.vector.tensor_tensor(out=fa, in0=ps[pbk][:, :], in1=fa, op=ALU.add),
                                     reads=[psb[pbk], faccb], writes=[faccb])
                first = False
        for t in range(nt):
            S.op("dve", lambda t=t: nc.vector.tensor_tensor(out=rr[:], in0=facc[:, t, :], in1=C.mod_bc[:, 1, isctx, :], op=ALU.mult),
                 reads=[faccb, C.mod_bcb], writes=[rrb])
            S.op("dve", lambda t=t: nc.vector.scalar_tensor_tensor(out=rr[:], in0=x1t[:, t, :], scalar=ALPHA, in1=rr[:], op0=ALU.mult, op1=ALU.add),
                 reads=[x1tb, rrb], writes=[rrb])
            ln_affine(C, rr, rrb, xt[:], xtb, lnp[:, 2, :], lnp[:, 3, :], lnpb, st6, stb)
            if l == 0:
                S.op("sp", lambda t=t, r0=r0: nc.sync.dma_start(out=Sx.x1[r0 + t * 128:r0 + (t + 1) * 128, :], in_=xt[:]),
                     reads=[xtb], writes=[dstb], dma=True)
            else:
                S.op("sp", lambda t=t, r0=r0: nc.sync.dma_start(out=C.y_out[r0 - TC + t * 128:r0 - TC + (t + 1) * 128, :], in_=xt[:]),
                     reads=[xtb], writes=[dstb], dma=True)


def ln_affine(C, src, srcb, dst, dstb, g, b, gbb, st6, stb):
    nc, S = C.nc, C.S
    for hh in range(2):
        S.op("dve", lambda hh=hh: nc.vector.bn_stats(out=st6[:, hh, :], in_=src[:, hh * 512:(hh + 1) * 512]), reads=[srcb], writes=[stb])
    S.op("dve", lambda: nc.vector.bn_aggr(out=st6[:, 2, 0:2], in_=st6[:, 0:2, :]), reads=[stb], writes=[stb])
    rstd_op(C, st6[:, 2, 2:3], st6[:, 2, 1:2], stb)
    S.op("dve", lambda: nc.vector.tensor_scalar(out=src[:], in0=src[:], scalar1=st6[:, 2, 0:1], scalar2=st6[:, 2, 2:3],
                                                op0=ALU.subtract, op1=ALU.mult), reads=[srcb, stb], writes=[srcb])
    S.op("dve", lambda: nc.vector.tensor_tensor(out=src[:], in0=src[:], in1=g, op=ALU.mult), reads=[srcb, gbb], writes=[srcb])
    S.op("dve", lambda: nc.vector.tensor_tensor(out=dst, in0=src[:], in1=b, op=ALU.add), reads=[srcb, gbb], writes=[dstb])


def make_consts():
    c = np.zeros((128, 8, 128), np.float32)
    r = np.arange(128)[:, None]
    q = np.arange(128)[None, :]
    c[:, 0, :] = (r == q)
    c[:, 1, :] = 1.0
    c[:, 2, :] = (r <= q)
    c[:, 3, :] = (r >= q)
    c[:, 4, :] = (r > q)
    c[:, 5, :] = (r < q)
    return c


def rowbc(v):
    v = np.asarray(v, np.float32).reshape(1, -1)
    return np.ascontiguousarray(np.broadcast_to(v, (128, v.shape[1])))


def prep_core_inputs(inp, b, half, TL, ne=NE, keys=None):
    f32 = lambda a: np.ascontiguousarray(np.asarray(a, dtype=np.float32))
    rev = (half == 1)
    dirs = [1, 0] if rev else [0, 1]

    def k_x():
        x_lat = np.asarray(inp["x"][b][:TL])
        x_ctx = np.asarray(inp["ctx"][b])
        if rev:
            x_lat = x_lat[::-1]
            x_ctx = x_ctx[::-1]
        return f32(np.concatenate([x_ctx, x_lat], axis=0))

    def k_sc():
        sc = np.stack([np.asarray(inp["c"][b]), np.asarray(inp["c_ctx"])], axis=-1)
        return f32(sc.reshape(8, 128, 2).transpose(1, 0, 2))

    def k_w_in():
        w_in = np.asarray(inp["w_in"])
        if rev:
            w_in = w_in.copy()
            tmp = w_in[:, :, 3072:3080].copy()
            w_in[:, :, 3072:3080] = w_in[:, :, 3080:3088]
            w_in[:, :, 3080:3088] = tmp
        return f32(w_in)

    def k_sgu_wT():
        sw = np.asarray(inp["sgu_w"])
        if rev:
            sw = sw[:, :, ::-1, ::-1]
        return f32(sw.transpose(0, 3, 1, 2))

    def k_sgu_b():
        sbias = np.asarray(inp["sgu_b"])
        if rev:
            sbias = sbias[:, :, ::-1]
        return f32(np.stack([rowbc(sbias[l].reshape(-1)).reshape(128, 4, 128) for l in range(2)]))

    def k_conv_w():
        cw = np.asarray(inp["conv_w"])
        if rev:
            cw = cw[:, ::-1, :]
        return f32(cw.reshape(2, 5, 12, 128).transpose(0, 3, 2, 1))

    def k_gate_par():
        gp = []
        for l in range(2):
            al = np.concatenate([np.asarray(inp["a_log"][l][d]) for d in dirs])
            db = np.concatenate([np.asarray(inp["dt_bias"][l][d]) for d in dirs])
            gp.append(np.stack([rowbc(al), rowbc(db)], axis=1))
        return f32(np.stack(gp))

    b_ada = lambda: np.asarray(inp["b_ada"])
    th = {
        "x": k_x, "sc": k_sc, "w_ada": lambda: f32(inp["w_ada"]),
        "b_ada_fm": lambda: f32(b_ada().reshape(2, 48, 128).transpose(0, 2, 1)),
        "b_ada_bc": lambda: f32(np.stack([rowbc(b_ada()[l]) for l in range(2)])),
        "w_in": k_w_in,
        "sgu_ln": lambda: f32(np.stack([np.stack([rowbc(inp["sgu_ln_g"][l]), rowbc(inp["sgu_ln_b"][l])], axis=1) for l in range(2)])),
        "sgu_wT": k_sgu_wT, "sgu_b": k_sgu_b, "conv_w": k_conv_w, "gate_par": k_gate_par,
        "gdn_nw": lambda: f32(np.stack([rowbc(np.tile(np.asarray(inp["gdn_norm_w"][l]), 4)) for l in range(2)])),
        "w_pa": lambda: f32(inp["w_pa"]), "w_pb": lambda: f32(inp["w_pb"]), "w_o": lambda: f32(inp["w_o"]),
        "lnp": lambda: f32(np.stack([np.stack([rowbc(inp[k][l]) for k in ("ln1_g", "ln1_b", "ln2_g", "ln2_b")], axis=1) for l in range(2)])),
        "ffn_w1": lambda: f32(inp["ffn_w1"][0]), "ffn_w3": lambda: f32(inp["ffn_w3"][0]), "ffn_w2": lambda: f32(inp["ffn_w2"][0]),
        "router": lambda: f32(np.asarray(inp["moe_router"][0]).reshape(8, 128, NE).transpose(1, 0, 2)),
        "moe_w1": lambda: f32(inp["moe_w1"][0][:ne]), "moe_w3": lambda: f32(inp["moe_w3"][0][:ne]),
        "moe_w2": lambda: f32(inp["moe_w2"][0][:ne]),
        "consts": make_consts,
    }
    if keys is None:
        keys = list(th)
    return {k: th[k]() for k in keys}


_CACHE = {}
FUSED = True


def get_programs(TL, ne=NE):
    key = (TL, ne, FUSED)
    if key not in _CACHE:
        if FUSED:
            _CACHE[key] = [build(TL, ne=ne)]
        else:
            _CACHE[key] = [build(TL, ne=ne, plan=p) for p in PLANS]
    return _CACHE[key]


def kernel(**inputs):
    x = np.asarray(inputs["x"])
    B, TL, _ = x.shape
    progs = get_programs(TL)
    ncores = 2 * B
    hand = [dict() for _ in range(ncores)]
    shared_cache = {}
    res = None
    for (nc, C) in progs:
        in_maps = []
        for b in range(B):
            for half in range(2):
                core = 2 * b + half
                keys = list(C.used_in)
                m = {}
                for k in keys:
                    if k in ("w_ada", "b_ada_fm", "b_ada_bc", "sgu_ln", "gdn_nw", "w_pa", "w_pb", "w_o", "lnp",
                             "ffn_w1", "ffn_w3", "ffn_w2", "router", "moe_w1", "moe_w3", "moe_w2", "consts"):
                        if k not in shared_cache:
                            shared_cache[k] = prep_core_inputs(inputs, b, half, TL, keys=[k])[k]
                        m[k] = shared_cache[k]
                    else:
                        m[k] = prep_core_inputs(inputs, b, half, TL, keys=[k])[k]
                for key in C.ext_in:
                    m["s_" + key] = hand[core]["s_" + key]
                in_maps.append(m)
        res = run_bass_kernel_spmd(nc, in_maps, core_ids=list(range(ncores)))
        for core in range(ncores):
            hand[core] = {("s_" + key): np.asarray(res.results[core]["s_" + key]) for key in C.ext_out}
        shared_cache = {k: v for k, v in shared_cache.items() if k == "consts"}
    out = np.zeros((B, TL, D), np.float32)
    OWN = TL // 2
    for b in range(B):
        y0 = np.asarray(res.results[2 * b]["y"])
        y1 = np.asarray(res.results[2 * b + 1]["y"])
        out[b, :OWN] = y0
        out[b, OWN:] = y1[::-1]
    return out
```
